# Optimizing a Trainium2 kernel written in Bass

```python
import jax, jax.numpy as jnp
from jax import lax
import numpy as np

D_MODEL = 1024
BATCH = 16
SEQ = 2048
DEPTH = 4

N_EVEN = (DEPTH + 1) // 2
N_ODD = DEPTH // 2
D_FF = 2816
NORM_EPS = 1e-6

ATTN_HEADS = 8
HEAD_DIM = 64
ATTN_WIDTH = ATTN_HEADS * HEAD_DIM
ROT_DIM = HEAD_DIM // 4
ROPE_THETA = 500000.0
MOBA_BLOCK = 256
MOBA_TOPK = 3
MOBA_Q_CHUNK = 32

LRU_WIDTH = 512
LRU_HEADS = 8
LRU_HEAD_DIM = LRU_WIDTH // LRU_HEADS
LRU_CONV = 4
LRU_C = 8.0

AB_IN = 3 * ATTN_WIDTH + 2 * LRU_WIDTH
AB_OUT = ATTN_WIDTH + LRU_WIDTH

SC_WIDTH = D_MODEL
SC_CONV = 3

kernel_name = "hybrid_rglru_moba_shortconv_macaron"


def rms_norm(x, g):
    xf = x.astype(jnp.float32)
    y = xf * lax.rsqrt(jnp.mean(xf * xf, axis=-1, keepdims=True) + NORM_EPS) * g.astype(jnp.float32)
    return y.astype(x.dtype)


def swiglu(h, w_gate, w_up, w_down):
    return (jax.nn.silu(h @ w_gate) * (h @ w_up)) @ w_down


def causal_depthwise_conv(x, w):
    k, c = w.shape
    return lax.conv_general_dilated(
        x, w[:, None, :].astype(x.dtype), window_strides=(1,), padding=[(k - 1, 0)],
        dimension_numbers=("NWC", "WIO", "NWC"), feature_group_count=c)


def partial_rope(x, pos):
    half = ROT_DIM // 2
    inv_freq = ROPE_THETA ** (-jnp.arange(0, ROT_DIM, 2, dtype=jnp.float32) / ROT_DIM)
    ang = pos[:, None] * inv_freq[None, :]
    cos = jnp.cos(ang)[None, :, None, :]
    sin = jnp.sin(ang)[None, :, None, :]
    xf = x.astype(jnp.float32)
    x1, x2, rest = xf[..., :half], xf[..., half:ROT_DIM], xf[..., ROT_DIM:]
    out = jnp.concatenate([x1 * cos - x2 * sin, x2 * cos + x1 * sin, rest], axis=-1)
    return out.astype(x.dtype)


def moba_attention(q, k, v):
    b, s, h, hd = q.shape
    n_blk = -(-s // MOBA_BLOCK)
    sp = n_blk * MOBA_BLOCK
    pad = ((0, 0), (0, sp - s), (0, 0), (0, 0))
    qt = jnp.pad(q, pad).transpose(0, 2, 1, 3)
    kt = jnp.pad(k, pad).transpose(0, 2, 1, 3)
    vt = jnp.pad(v, pad).transpose(0, 2, 1, 3)
    kb = kt.reshape(b, h, n_blk, MOBA_BLOCK, hd)
    vb = vt.reshape(b, h, n_blk, MOBA_BLOCK, hd)
    kmean = jnp.mean(kb.astype(jnp.float32), axis=3)
    n_sel = min(MOBA_TOPK, n_blk - 1)
    scale = hd ** -0.5
    blk_ids = jnp.arange(n_blk)
    bi = jnp.arange(b)[:, None, None]
    hi = jnp.arange(h)[None, :, None]
    neg = jnp.float32(-1e30)

    def chunk(c):
        start = c * MOBA_Q_CHUNK
        own = start // MOBA_BLOCK
        qc = lax.dynamic_slice_in_dim(qt, start, MOBA_Q_CHUNK, axis=2)
        qpos = start + jnp.arange(MOBA_Q_CHUNK)
        k_own = lax.dynamic_index_in_dim(kb, own, axis=2, keepdims=False)
        v_own = lax.dynamic_index_in_dim(vb, own, axis=2, keepdims=False)
        kpos = own * MOBA_BLOCK + jnp.arange(MOBA_BLOCK)
        s_own = jnp.einsum("bhqd,bhkd->bhqk", qc, k_own).astype(jnp.float32) * scale
        s_own = jnp.where(kpos[None, :] <= qpos[:, None], s_own, neg)
        logits = [s_own]
        vals = [v_own]
        if n_sel > 0:
            g = jnp.einsum("bhqd,bhnd->bhqn", qc.astype(jnp.float32), kmean)
            g = jnp.where(blk_ids < own, g, neg)
            _, idx = lax.top_k(g, n_sel)
            valid = idx < own
            for j in range(n_sel):
                ks = kb[bi, hi, idx[..., j]]
                vs = vb[bi, hi, idx[..., j]]
                sj = jnp.einsum("bhqd,bhqkd->bhqk", qc, ks).astype(jnp.float32) * scale
                logits.append(jnp.where(valid[..., j][..., None], sj, neg))
                vals.append(vs)
        p = jax.nn.softmax(jnp.concatenate(logits, axis=-1), axis=-1)
        p = p.astype(v.dtype)
        out = jnp.einsum("bhqk,bhkd->bhqd", p[..., :MOBA_BLOCK], vals[0])
        for j in range(1, n_sel + 1):
            pj = p[..., j * MOBA_BLOCK:(j + 1) * MOBA_BLOCK]
            out = out + jnp.einsum("bhqk,bhqkd->bhqd", pj, vals[j])
        return out

    outs = lax.map(chunk, jnp.arange(sp // MOBA_Q_CHUNK))
    outs = outs.transpose(1, 0, 3, 2, 4).reshape(b, sp, h, hd)[:, :s]
    return outs.reshape(b, s, h * hd)


def rg_lru(xc, wa, ba, wx, bx, lam):
    b, s, w = xc.shape
    xf = xc.astype(jnp.float32).reshape(b, s, LRU_HEADS, LRU_HEAD_DIM)
    r = jax.nn.sigmoid(jnp.einsum("bshi,hij->bshj", xf, wa.astype(jnp.float32)) + ba.astype(jnp.float32))
    i = jax.nn.sigmoid(jnp.einsum("bshi,hij->bshj", xf, wx.astype(jnp.float32)) + bx.astype(jnp.float32))
    log_a = -LRU_C * r * jax.nn.softplus(-lam.astype(jnp.float32)).reshape(LRU_HEADS, LRU_HEAD_DIM)
    a = jnp.exp(log_a)
    u = jnp.sqrt(-jnp.expm1(2.0 * log_a)) * (i * xf)
    a = a.reshape(b, s, w)
    u = u.reshape(b, s, w)

    def combine(e1, e2):
        a1, u1 = e1
        a2, u2 = e2
        return a1 * a2, a2 * u1 + u2

    _, hseq = lax.associative_scan(combine, (a, u), axis=1)
    return hseq.astype(xc.dtype)


def setup_inputs(seed: int = 0) -> dict:
    key = jax.random.key(seed)
    ks = jax.random.split(key, 24)
    f32 = jnp.float32

    def nrm(k, shape, fan_in):
        return jax.random.normal(k, shape, f32) * (fan_in ** -0.5)

    x = jax.random.normal(ks[0], (BATCH, SEQ, D_MODEL), f32)
    ffn1_w_gate = nrm(ks[1], (DEPTH, D_MODEL, D_FF), D_MODEL)
    ffn1_w_up = nrm(ks[2], (DEPTH, D_MODEL, D_FF), D_MODEL)
    ffn1_w_down = nrm(ks[3], (DEPTH, D_FF, D_MODEL), D_FF)
    ffn2_w_gate = nrm(ks[4], (DEPTH, D_MODEL, D_FF), D_MODEL)
    ffn2_w_up = nrm(ks[5], (DEPTH, D_MODEL, D_FF), D_MODEL)
    ffn2_w_down = nrm(ks[6], (DEPTH, D_FF, D_MODEL), D_FF)
    norm_pre = 1.0 + 0.05 * jax.random.normal(ks[7], (DEPTH, 3, D_MODEL), f32)
    norm_post = 1.0 + 0.05 * jax.random.normal(ks[8], (DEPTH, 3, D_MODEL), f32)
    ab_w_in = nrm(ks[9], (N_EVEN, D_MODEL, AB_IN), D_MODEL)
    ab_w_out = nrm(ks[10], (N_EVEN, AB_OUT, D_MODEL), AB_OUT)
    lru_conv_w = nrm(ks[11], (N_EVEN, LRU_CONV, LRU_WIDTH), LRU_CONV)
    lru_conv_b = 0.01 * jax.random.normal(ks[12], (N_EVEN, LRU_WIDTH), f32)
    lru_gate_a_w = nrm(ks[13], (N_EVEN, LRU_HEADS, LRU_HEAD_DIM, LRU_HEAD_DIM), LRU_HEAD_DIM)
    lru_gate_a_b = 0.01 * jax.random.normal(ks[14], (N_EVEN, LRU_HEADS, LRU_HEAD_DIM), f32)
    lru_gate_x_w = nrm(ks[15], (N_EVEN, LRU_HEADS, LRU_HEAD_DIM, LRU_HEAD_DIM), LRU_HEAD_DIM)
    lru_gate_x_b = 0.01 * jax.random.normal(ks[16], (N_EVEN, LRU_HEADS, LRU_HEAD_DIM), f32)
    a_c = jax.random.uniform(ks[17], (N_EVEN, LRU_WIDTH), f32, 0.9, 0.999)
    a0 = a_c ** (1.0 / LRU_C)
    lru_lambda = jnp.log(a0) - jnp.log1p(-a0)
    c_w_in = nrm(ks[18], (N_ODD, D_MODEL, 3 * SC_WIDTH), D_MODEL)
    c_conv_w = nrm(ks[19], (N_ODD, SC_CONV, SC_WIDTH), SC_CONV)
    c_w_out = nrm(ks[20], (N_ODD, SC_WIDTH, D_MODEL), SC_WIDTH)
    return {
        "x": x,
        "ffn1_w_gate": ffn1_w_gate, "ffn1_w_up": ffn1_w_up, "ffn1_w_down": ffn1_w_down,
        "ffn2_w_gate": ffn2_w_gate, "ffn2_w_up": ffn2_w_up, "ffn2_w_down": ffn2_w_down,
        "norm_pre": norm_pre, "norm_post": norm_post,
        "ab_w_in": ab_w_in, "ab_w_out": ab_w_out,
        "lru_conv_w": lru_conv_w, "lru_conv_b": lru_conv_b,
        "lru_gate_a_w": lru_gate_a_w, "lru_gate_a_b": lru_gate_a_b,
        "lru_gate_x_w": lru_gate_x_w, "lru_gate_x_b": lru_gate_x_b,
        "lru_lambda": lru_lambda,
        "c_w_in": c_w_in, "c_conv_w": c_conv_w, "c_w_out": c_w_out,
    }


def reference(x, ffn1_w_gate, ffn1_w_up, ffn1_w_down, ffn2_w_gate, ffn2_w_up, ffn2_w_down,
              norm_pre, norm_post, ab_w_in, ab_w_out, lru_conv_w, lru_conv_b,
              lru_gate_a_w, lru_gate_a_b, lru_gate_x_w, lru_gate_x_b, lru_lambda,
              c_w_in, c_conv_w, c_w_out):
    b, s, d = x.shape
    pos = jnp.arange(s, dtype=jnp.float32)
    for l in range(DEPTH):
        hdn = swiglu(rms_norm(x, norm_pre[l, 0]), ffn1_w_gate[l], ffn1_w_up[l], ffn1_w_down[l])
        x = x + 0.5 * rms_norm(hdn, norm_post[l, 0])

        h = rms_norm(x, norm_pre[l, 1])
        if l % 2 == 0:
            e = l // 2
            p = h @ ab_w_in[e]
            q = p[..., :ATTN_WIDTH].reshape(b, s, ATTN_HEADS, HEAD_DIM)
            k = p[..., ATTN_WIDTH:2 * ATTN_WIDTH].reshape(b, s, ATTN_HEADS, HEAD_DIM)
            v = p[..., 2 * ATTN_WIDTH:3 * ATTN_WIDTH].reshape(b, s, ATTN_HEADS, HEAD_DIM)
            lru_x = p[..., 3 * ATTN_WIDTH:3 * ATTN_WIDTH + LRU_WIDTH]
            lru_g = p[..., 3 * ATTN_WIDTH + LRU_WIDTH:]
            attn = moba_attention(partial_rope(q, pos), partial_rope(k, pos), v)
            xc = causal_depthwise_conv(lru_x, lru_conv_w[e]) + lru_conv_b[e]
            rec = rg_lru(xc, lru_gate_a_w[e], lru_gate_a_b[e], lru_gate_x_w[e], lru_gate_x_b[e],
                         lru_lambda[e]) * jax.nn.gelu(lru_g)
            mix = jnp.concatenate([attn, rec], axis=-1) @ ab_w_out[e]
        else:
            o = l // 2
            p = h @ c_w_in[o]
            gate_b = p[..., :SC_WIDTH]
            gate_c = p[..., SC_WIDTH:2 * SC_WIDTH]
            xt = p[..., 2 * SC_WIDTH:]
            mix = (gate_b * causal_depthwise_conv(gate_c * xt, c_conv_w[o])) @ c_w_out[o]
        x = x + rms_norm(mix, norm_post[l, 1])

        hdn = swiglu(rms_norm(x, norm_pre[l, 2]), ffn2_w_gate[l], ffn2_w_up[l], ffn2_w_down[l])
        x = x + 0.5 * rms_norm(hdn, norm_post[l, 2])
    return x
```

```python
import numpy as np
from contextlib import ExitStack
import concourse.bass as bass
import concourse.mybir as mybir
from concourse.bass_utils import run_bass_kernel_spmd

F32 = mybir.dt.float32
BF16 = mybir.dt.bfloat16
AF = mybir.ActivationFunctionType
ALU = mybir.AluOpType
AX = mybir.AxisListType

ENGS = ("pe", "act", "dve", "pool", "sp")
NCORES = 8
SEQ = 2048
DM = 1024
DFF = 2816
NJ = 22
SEG = 1024
TT = 512
EPS = 1e-6
ROPE_THETA = 500000.0
WSLOT = 3072
N_WSLOT = 4


def M(name, *a, **kw):
    return (name, a, kw)


class Buf:
    __slots__ = ("name", "w", "r")

    def __init__(self, name):
        self.name = name
        self.w = None
        self.r = []


class Op:
    __slots__ = ("eng", "fn", "deps", "mark", "semval", "is_dma", "dma_sem", "dma_val", "idx", "phase")


class Prog:
    def __init__(self, nc, per_q=10):
        self.nc = nc
        self.ops = {e: [] for e in ENGS}
        self.per_q = per_q
        self.dma_q = {"sp": 0, "act": 1, "pool": 2}
        self.n_dma_sems = per_q * 3
        self.dma_sem_next = {"sp": 0, "act": 0, "pool": 0}
        self.dma_counts = [0] * self.n_dma_sems
        self.all_ops = []
        self.annotate = getattr(Prog, 'annotate_default', False)
        self.phase = ""

    def _add(self, eng, fn, reads, writes, is_dma=False, accum=False):
        op = Op()
        op.eng = eng
        op.fn = fn
        op.mark = False
        op.semval = None
        op.is_dma = is_dma
        op.dma_sem = None
        op.dma_val = None
        op.idx = len(self.ops[eng])
        op.phase = getattr(self, "phase", "")
        deps = {}
        for b in reads:
            if b.w is not None:
                deps[id(b.w)] = b.w
        for b in writes:
            if b.w is not None:
                if not (accum and b.w.eng == "pe" and eng == "pe" and not b.w.is_dma):
                    deps[id(b.w)] = b.w
            for r in b.r:
                deps[id(r)] = r
        deps.pop(id(op), None)
        op.deps = [d for d in deps.values()
                   if not (d.eng == "pe" and eng == "pe" and not d.is_dma and not is_dma)]
        for b in reads:
            b.r.append(op)
        for b in writes:
            b.w = op
            b.r = []
        if is_dma:
            k = self.dma_sem_next[eng]
            self.dma_sem_next[eng] = (k + 1) % self.per_q
            s = self.dma_q[eng] * self.per_q + k
            self.dma_counts[s] += 16
            op.dma_sem = s
            op.dma_val = self.dma_counts[s]
        self.ops[eng].append(op)
        self.all_ops.append(op)
        return op

    def pe(self, fn, reads=(), writes=(), accum=False):
        return self._add("pe", fn, reads, writes, accum=accum)

    def act(self, fn, reads=(), writes=()):
        return self._add("act", fn, reads, writes)

    def dve(self, fn, reads=(), writes=()):
        return self._add("dve", fn, reads, writes)

    def pool(self, fn, reads=(), writes=()):
        return self._add("pool", fn, reads, writes)

    def dma(self, eng, fn, reads=(), writes=()):
        return self._add(eng, fn, reads, writes, is_dma=True)

    def emit(self, final_wait_ops=()):
        nc = self.nc
        for op in self.all_ops:
            for d in op.deps:
                if not d.is_dma:
                    d.mark = True
        for op in final_wait_ops:
            if not op.is_dma:
                op.mark = True
        cnt = {e: 0 for e in ENGS}
        for e in ENGS:
            for op in self.ops[e]:
                if op.mark and not op.is_dma:
                    cnt[e] += 1
                    op.semval = cnt[e]
        self.stats = {}
        with ExitStack() as st:
            esem = {e: st.enter_context(nc.semaphore("es_" + e)) for e in ENGS}
            dsem = [st.enter_context(nc.semaphore("ds%d" % i)) for i in range(self.n_dma_sems)]
            block = st.enter_context(nc.Block())

            def run(engname, engobj):
                known_e = {e: 0 for e in ENGS}
                known_d = [0] * self.n_dma_sems
                nwait = 0
                for op in self.ops[engname]:
                    need_e = {}
                    need_d = {}
                    for d in op.deps:
                        if d.is_dma:
                            if d.dma_val > known_d[d.dma_sem]:
                                if d.dma_val > need_d.get(d.dma_sem, 0):
                                    need_d[d.dma_sem] = d.dma_val
                        else:
                            if d.semval > known_e[d.eng]:
                                if d.semval > need_e.get(d.eng, 0):
                                    need_e[d.eng] = d.semval
                    if op.is_dma and op.dma_val - 16 > known_d[op.dma_sem]:
                        if op.dma_val - 16 > need_d.get(op.dma_sem, 0):
                            need_d[op.dma_sem] = op.dma_val - 16
                    for e, v in need_e.items():
                        engobj.wait_ge(esem[e], v)
                        known_e[e] = v
                        nwait += 1
                    for s, v in need_d.items():
                        engobj.wait_ge(dsem[s], v)
                        known_d[s] = v
                        nwait += 1
                    ins = getattr(engobj, op.fn[0])(*op.fn[1], **op.fn[2])
                    if self.annotate and op.phase:
                        ins.annotate(op.phase)
                    if op.is_dma:
                        ins.then_inc(dsem[op.dma_sem], 16)
                    elif op.mark:
                        ins.then_inc(esem[engname], 1)
                if engname == "sp":
                    for op in final_wait_ops:
                        if op.is_dma:
                            if op.dma_val > known_d[op.dma_sem]:
                                engobj.wait_ge(dsem[op.dma_sem], op.dma_val)
                                known_d[op.dma_sem] = op.dma_val
                        else:
                            if op.semval > known_e[op.eng]:
                                engobj.wait_ge(esem[op.eng], op.semval)
                                known_e[op.eng] = op.semval
                self.stats[engname] = (len(self.ops[engname]), nwait)

            @block.tensor
            def _(eng):
                run("pe", eng)

            @block.scalar
            def _(eng):
                run("act", eng)

            @block.vector
            def _(eng):
                run("dve", eng)

            @block.gpsimd
            def _(eng):
                run("pool", eng)

            @block.sync
            def _(eng):
                run("sp", eng)


def wlayout():
    off = 0
    L = {}

    def add(key, n):
        nonlocal off
        L[key] = (off, n)
        off += n
    for l in range(4):
        for f in range(2):
            for j in range(NJ):
                add(("gu", l, f, j), 2048)
            for c in range(8):
                add(("d", l, f, c), 2816)
        if l % 2 == 0:
            e = l // 2
            for c in range(4):
                add(("lru", e, c), 2048)
            for c in range(4):
                add(("qk", e, c), 2048)
            for vh in range(2):
                add(("v", e, vh), 2048)
        else:
            o = l // 2
            for c in range(8):
                add(("cin", o, c), 3072)
        for g in range(4):
            add(("out", l, g), 2048)
    return L, off


def playout():
    off = 0
    L = {}

    def add(key, n=1):
        nonlocal off
        L[key] = off
        off += n
    for l in range(4):
        for w in range(3):
            add(("pre", l, w), 8)
            add(("post", l, w), 8)
    for e in range(2):
        for j in range(4):
            add(("lcw", e, j), 4)
        add(("lcb", e), 4)
        add(("ba", e), 4)
        add(("bx", e), 4)
        add(("lam", e), 4)
    for o in range(2):
        for j in range(3):
            add(("ccw", o, j), 8)
    return L, off


def _chunkT(W, col):
    return W[:, col:col + 128].reshape(8, 128, 128).transpose(1, 0, 2)


def pack_weights(inp):
    L, tot = wlayout()
    wall = np.empty((128, tot), np.float32)

    def put(key, arr):
        o, n = L[key]
        wall[:, o:o + n] = arr.reshape(128, n)
    for l in range(4):
        for f in range(2):
            if f == 0:
                wg, wu, wd = inp["ffn1_w_gate"][l], inp["ffn1_w_up"][l], inp["ffn1_w_down"][l]
            else:
                wg, wu, wd = inp["ffn2_w_gate"][l], inp["ffn2_w_up"][l], inp["ffn2_w_down"][l]
            for j in range(NJ):
                put(("gu", l, f, j), np.stack([_chunkT(wg, j * 128), _chunkT(wu, j * 128)], axis=1))
            for c in range(8):
                put(("d", l, f, c), wd[:, c * 128:(c + 1) * 128].reshape(NJ, 128, 128).transpose(1, 0, 2))
        if l % 2 == 0:
            e = l // 2
            win = inp["ab_w_in"][e]
            wout = inp["ab_w_out"][e]
            for c in range(4):
                put(("lru", e, c), np.stack([_chunkT(win, 2048 + c * 128), _chunkT(win, 1536 + c * 128)], axis=1))
                put(("qk", e, c), np.stack([_chunkT(win, c * 128), _chunkT(win, 512 + c * 128)], axis=1))
            for vh in range(2):
                put(("v", e, vh), win[:, 1024 + vh * 256:1024 + (vh + 1) * 256].reshape(8, 128, 256).transpose(1, 0, 2))
        else:
            o = l // 2
            win = inp["c_w_in"][o]
            wout = inp["c_w_out"][o]
            for c in range(8):
                put(("cin", o, c), np.stack([_chunkT(win, 1024 + c * 128), _chunkT(win, 2048 + c * 128),
                                             _chunkT(win, c * 128)], axis=1))
        for g in range(4):
            put(("out", l, g), np.stack([_chunkT(wout, (2 * g) * 128), _chunkT(wout, (2 * g + 1) * 128)], axis=1))
    return wall


def pack_small(inp):
    L, n = playout()
    pv = np.zeros((128, n), np.float32)

    def colmajor(v):
        return v.reshape(-1, 128).T
    for l in range(4):
        for w in range(3):
            pv[:, L[("pre", l, w)]:L[("pre", l, w)] + 8] = colmajor(inp["norm_pre"][l, w])
            pv[:, L[("post", l, w)]:L[("post", l, w)] + 8] = colmajor(inp["norm_post"][l, w])
    for e in range(2):
        for j in range(4):
            pv[:, L[("lcw", e, j)]:L[("lcw", e, j)] + 4] = colmajor(inp["lru_conv_w"][e, j])
        pv[:, L[("lcb", e)]:L[("lcb", e)] + 4] = colmajor(inp["lru_conv_b"][e])
        pv[:, L[("ba", e)]:L[("ba", e)] + 4] = colmajor(inp["lru_gate_a_b"][e].reshape(-1))
        pv[:, L[("bx", e)]:L[("bx", e)] + 4] = colmajor(inp["lru_gate_x_b"][e].reshape(-1))
        pv[:, L[("lam", e)]:L[("lam", e)] + 4] = colmajor(inp["lru_lambda"][e])
    for o in range(2):
        for j in range(3):
            pv[:, L[("ccw", o, j)]:L[("ccw", o, j)] + 8] = colmajor(inp["c_conv_w"][o, j])
    wsm = np.zeros((128, 16, 128), np.float32)
    for e in range(2):
        for gi, nm in enumerate(("lru_gate_a_w", "lru_gate_x_w")):
            w = inp[nm][e]
            for c in range(4):
                idx = (e * 2 + gi) * 4 + c
                wsm[0:64, idx, 0:64] = w[2 * c]
                wsm[64:128, idx, 64:128] = w[2 * c + 1]
    return pv, wsm.reshape(128, 16 * 128)


def const_tables():
    cm = np.zeros((128, 4, 128), np.float32)
    cm[:, 0, :] = np.eye(128, dtype=np.float32)
    cm[:, 1, :] = 1.0
    for m in range(128):
        j = m % 64
        if j < 8:
            cm[m + 8, 2, m] = 1.0
        elif j < 16:
            cm[m - 8, 2, m] = 1.0
    k = np.arange(128)
    cm[:, 3, :] = (k[:, None] <= k[None, :]).astype(np.float32)
    inv_freq = (np.float32(ROPE_THETA) ** (-np.arange(0, 16, 2, dtype=np.float32) / np.float32(16))).astype(np.float32)
    pos = np.arange(SEQ, dtype=np.float32)
    ang = (pos[:, None] * inv_freq[None, :]).astype(np.float32).astype(np.float64)
    rope = np.zeros((128, 2, SEQ), np.float32)
    rope[:, 0, :] = 1.0
    for p in range(128):
        j = p % 64
        if j < 16:
            i = j % 8
            rope[p, 0, :] = np.cos(ang[:, i])
            rope[p, 1, :] = (-np.sin(ang[:, i])) if j < 8 else np.sin(ang[:, i])
    return cm.reshape(128, 4 * 128), rope.reshape(128, 2 * SEQ)


class Builder:
    def __init__(self, nseq=2, layers=(0, 1, 2, 3), nhalf=2, dbg=False, parts=("ffn1", "mixer", "ffn2")):
        self.dbg = dbg
        self.parts = parts
        self.stop_after = None
        self.wtot_override = None
        self.nseq = nseq
        self.layers = tuple(layers)
        self.nhalf = nhalf
        self.WL, self.WTOT = wlayout()
        self.PL, self.NP = playout()

    def sb(self, name, shape, dt):
        return self.st.enter_context(self.nc.sbuf_tensor(name, shape, dt))

    def ps(self, hold=False):
        while True:
            i = self.ps_set[self.ps_next % len(self.ps_set)]
            self.ps_next += 1
            if i not in self.ps_held:
                break
        if hold:
            self.ps_held.add(i)
        return self.PS[i], self.bPS[i]

    def ps_release(self, t):
        for i in range(8):
            if self.PS[i] is t:
                self.ps_held.discard(i)

    def rot(self, poolname):
        tiles, bufs, st = self.pools[poolname]
        i = st[0] % len(tiles)
        st[0] += 1
        return tiles[i], bufs[i]

    def mkpool(self, name, n, shape, dt):
        tiles = [self.sb("%s%d" % (name, i), shape, dt) for i in range(n)]
        bufs = [Buf("%s%d" % (name, i)) for i in range(n)]
        self.pools[name] = (tiles, bufs, [0])

    def wload(self, key):
        off, n = self.WL[key]
        i = self.w_next % N_WSLOT
        self.w_next += 1
        t, b = self.WS[i], self.bWS[i]
        self.P.dma("pool", M("dma_start", out=t[:, 0:n], in_=self.wall[:, off:off + n]),
                   writes=[b])
        return t, b

    def pcol(self, key, c=0):
        o = self.PL[key] + c
        return self.pvec[:, o:o + 1]

    def arena_reset(self):
        last = {}
        dmas = []
        for b in self.arena_bufs:
            ops = list(b.r)
            if b.w is not None:
                ops.append(b.w)
            for op in ops:
                if op.is_dma:
                    dmas.append(op)
                else:
                    cur = last.get(op.eng)
                    if cur is None or op.idx > cur.idx:
                        last[op.eng] = op
        self.arena_haz = list(last.values()) + dmas
        self.arena_bufs = []

    def abuf(self, name):
        b = Buf(name)
        b.r = list(self.arena_haz)
        self.arena_bufs.append(b)
        return b

    def build(self):
        nc = bass.Bass("TRN2", target_bir_lowering=False)
        self.nc = nc
        self.xs = nc.dram_tensor("xs", [self.nseq, SEQ, DM], F32, kind="ExternalInput").ap()
        self.wall = nc.dram_tensor("wall", [128, self.wtot_override or self.WTOT], F32, kind="ExternalInput").ap()
        self.pv_d = nc.dram_tensor("pv_in", [128, self.NP], F32, kind="ExternalInput").ap()
        self.wsm_d = nc.dram_tensor("wsm_in", [128, 2048], F32, kind="ExternalInput").ap()
        self.cm_d = nc.dram_tensor("cm_in", [128, 512], F32, kind="ExternalInput").ap()
        self.rope_d = nc.dram_tensor("rope_in", [128, 2 * SEQ], F32, kind="ExternalInput").ap()
        self.ys = nc.dram_tensor("ys", [self.nseq, SEQ, DM], F32, kind="ExternalOutput").ap()
        if self.dbg:
            self.dbg_d = nc.dram_tensor("dbg", [2, 128, 8 * SEG], F32, kind="ExternalOutput").ap()
        self.P = Prog(nc)
        self.pools = {}
        self.finals = []
        with ExitStack() as st:
            self.st = st
            self.alloc()
            self.setup()
            for s in range(self.nseq):
                self.seq_reset()
                for half in range(self.nhalf):
                    self.load_x(s, half)
                    sched = []
                    for l in self.layers:
                        if "ffn1" in self.parts:
                            sched.append(("ffn", l, 0))
                        if "mixer" in self.parts:
                            sched.append(("mix", l, 1))
                        if "ffn2" in self.parts:
                            sched.append(("ffn", l, 2))
                    if sched:
                        self.prenorm(sched[0][1], sched[0][2])
                    for i, (kind, l, w) in enumerate(sched):
                        self.next_norm = (sched[i + 1][1], sched[i + 1][2]) if i + 1 < len(sched) else None
                        if kind == "ffn":
                            self.ffn(l, w // 2, half)
                        elif l % 2 == 0:
                            self.mixer_even(l, half)
                        else:
                            self.mixer_odd(l, half)
                    self.store_x(s, half)
            self.P.emit(final_wait_ops=self.finals)
        return nc

    def alloc(self):
        nc = self.nc
        sb = self.sb
        self.X = sb("X", [128, 8, SEG], F32)
        self.bX = [[Buf("X%d_%d" % (c, t)) for t in range(2)] for c in range(8)]
        self.KTs = [sb("KTs%d" % e, [128, 4, SEG], BF16) for e in range(2)]
        self.bKTs = [[Buf("KTs%d_%d" % (e, c)) for c in range(4)] for e in range(2)]
        self.Vs = [sb("Vs%d" % e, [128, 8, 520], BF16) for e in range(2)]
        self.bVs = [[Buf("Vs%d_%d" % (e, k)) for k in range(8)] for e in range(2)]
        self.pvec = sb("pvec", [128, self.NP], F32)
        self.bpv = Buf("pvec")
        self.wsm = sb("wsm", [128, 16, 128], BF16)
        self.bwsm = Buf("wsm")
        self.cb = sb("cb", [128, 4, 128], BF16)
        self.bcb = Buf("cb")
        self.identf = sb("identf", [128, 128], F32)
        self.bident = Buf("identf")
        self.lc = sb("lc", [128, 2, 4, 2], F32)
        self.blc = Buf("lc")
        self.zh = sb("zh", [128, 2, 8, 2], F32)
        self.bzh = [[Buf("zh%d_%d" % (o, c)) for c in range(8)] for o in range(2)]
        self.lh = sb("lh", [128, 2, 4, 4], F32)
        self.blh = [[Buf("lh%d_%d" % (e, c)) for c in range(4)] for e in range(2)]
        self.hst = sb("hst", [128, 2, 4], F32)
        self.bhst = [[Buf("hst%d_%d" % (e, c)) for c in range(4)] for e in range(2)]
        self.kmx = sb("kmx", [128, 2, 4], F32)
        self.bkmx = [Buf("kmx%d" % e) for e in range(2)]
        self.small = sb("small", [128, 64], F32)
        self.H = sb("H", [128, 8, SEG], BF16)
        self.Ysb = self.H[:].rearrange("p c t -> p (c t)").bitcast(F32)
        self.bH = [[Buf("H%d_%d" % (c, t)) for t in range(2)] for c in range(8)]
        self.WS = [sb("ws%d" % i, [128, WSLOT], BF16) for i in range(N_WSLOT)]
        self.bWS = [Buf("ws%d" % i) for i in range(N_WSLOT)]
        self.w_next = 0
        self.mkpool("t32", 5, [128, TT], F32)
        self.mkpool("tb", 4, [128, TT], BF16)
        self.mkpool("rs", 2, [128, TT], F32)
        self.AR_BYTES = 74 * 1024
        self.AR = sb("arena", [128, self.AR_BYTES // 2], BF16)
        self.arena_bufs = []
        self.arena_haz = []
        self.PS = [self.st.enter_context(nc.psum_tensor("ps%d" % i, [128, TT], F32)) for i in range(8)]
        self.bPS = [Buf("ps%d" % i) for i in range(8)]
        self.ps_set = list(range(8))
        self.ps_next = 0
        self.ps_held = set()
        self.pending_stats = []

    def arena_view(self, byte_off, shape, dt):
        n = int(np.prod(shape[1:]))
        esz = 2 if dt == BF16 else 4
        assert byte_off % 4 == 0
        assert byte_off + n * esz <= self.AR_BYTES, (byte_off, n, esz)
        v = self.AR[:, byte_off // 2: byte_off // 2 + n * esz // 2]
        if dt == F32:
            v = v.bitcast(F32)
        return v

    def setup(self):
        P = self.P
        P.dma("sp", M("dma_start", out=self.pvec[:], in_=self.pv_d), writes=[self.bpv])
        P.dma("sp", M("dma_start", out=self.identf[:], in_=self.cm_d[:, 0:128]), writes=[self.bident])
        P.dma("pool", M("dma_start", out=self.wsm[:], in_=self.wsm_d.rearrange("p (a b) -> p a b", a=16)),
              writes=[self.bwsm])
        P.dma("pool", M("dma_start", out=self.cb[:], in_=self.cm_d.rearrange("p (a b) -> p a b", a=4)),
              writes=[self.bcb])
        bsm = Buf("small")
        for e in range(2):
            lam = self.pvec[:, self.PL[("lam", e)]:self.PL[("lam", e)] + 4]
            t1 = self.small[:, 0:4]
            t2 = self.small[:, 4:8]
            P.act(M("activation", out=t1, in_=lam, func=AF.Exp, scale=-1.0),
                  reads=[self.bpv], writes=[bsm])
            P.act(M("activation", out=t2, in_=t1, func=AF.Ln, bias=1.0),
                  reads=[bsm], writes=[bsm])
            P.dve(M("tensor_scalar", out=self.lc[:, e, :, 0], in0=t2, scalar1=-8.0, scalar2=None,
                                                         op0=ALU.mult), reads=[bsm], writes=[self.blc])
            P.dve(M("tensor_scalar", out=self.lc[:, e, :, 1], in0=t2, scalar1=-16.0, scalar2=None,
                                                         op0=ALU.mult), reads=[bsm], writes=[self.blc])
        for e in range(2):
            v4 = self.Vs[e][:].rearrange("p k (h d) -> p k h d", h=8)
            P.dve(M("memset", v4[:, :, :, 64:65], 1.0), writes=self.bVs[e])

    def seq_reset(self):
        P = self.P
        for o in range(2):
            P.dve(M("memset", self.zh[:, o], 0.0), writes=self.bzh[o])
        for e in range(2):
            P.dve(M("memset", self.lh[:, e], 0.0), writes=self.blh[e])
            P.dve(M("memset", self.hst[:, e], 0.0), writes=self.bhst[e])

    def stage_tiles(self):
        self.arena_reset()
        tiles = [self.arena_view(i * 4096, [128, DM], F32) for i in range(2)]
        bufs = [self.abuf("stage%d" % i) for i in range(2)]
        return tiles, bufs

    def load_x(self, s, half):
        P = self.P
        P.phase = "io"
        stiles, sbufs = self.stage_tiles()
        for tk in range(8):
            t0 = half * SEG + tk * 128
            stg, bst = stiles[tk % 2], sbufs[tk % 2]
            P.dma("sp", M("dma_start", out=stg, in_=self.xs[s, t0:t0 + 128, :]), writes=[bst])
            for cg in range(2):
                pt, bpt = self.ps()
                for ci in range(4):
                    c = cg * 4 + ci
                    P.pe(M("transpose", out=pt[:, ci * 128:(ci + 1) * 128],
                                                                         in_=stg[:, c * 128:(c + 1) * 128],
                                                                         identity=self.identf[:]),
                         reads=[bst, self.bident], writes=[bpt], accum=(ci > 0))
                tt = tk // 4
                col = tk * 128
                eng = P.act if cg == 0 else P.dve
                if cg == 0:
                    P.act(M("activation",
                        out=self.X[:, cg * 4:(cg + 1) * 4, col:col + 128],
                        in_=pt[:].rearrange("p (a b) -> p a b", a=4), func=AF.Copy),
                        reads=[bpt], writes=[self.bX[c][tt] for c in range(cg * 4, cg * 4 + 4)])
                else:
                    P.dve(M("tensor_copy",
                        out=self.X[:, cg * 4:(cg + 1) * 4, col:col + 128],
                        in_=pt[:].rearrange("p (a b) -> p a b", a=4)),
                        reads=[bpt], writes=[self.bX[c][tt] for c in range(cg * 4, cg * 4 + 4)])

    def store_x(self, s, half):
        P = self.P
        P.phase = "io"
        stiles, sbufs = self.stage_tiles()
        for tk in range(8):
            t0 = half * SEG + tk * 128
            tt = tk // 4
            col = tk * 128
            stg, bst = stiles[tk % 2], sbufs[tk % 2]
            for cg in range(2):
                pt, bpt = self.ps()
                for ci in range(4):
                    c = cg * 4 + ci
                    P.pe(M("transpose", out=pt[:, ci * 128:(ci + 1) * 128],
                                                                         in_=self.X[:, c, col:col + 128],
                                                                         identity=self.identf[:]),
                         reads=[self.bX[c][tt], self.bident], writes=[bpt], accum=(ci > 0))
                if cg == 0:
                    P.act(M("activation", out=stg[:, 0:512], in_=pt[:], func=AF.Copy),
                          reads=[bpt], writes=[bst])
                else:
                    P.dve(M("tensor_copy", out=stg[:, 512:1024], in_=pt[:]),
                          reads=[bpt], writes=[bst])
            d = P.dma("sp", M("dma_start", out=self.ys[s, t0:t0 + 128, :], in_=stg), reads=[bst])
            self.finals.append(d)

    def rstd_from(self, sps, bsps, hw):
        P = self.P
        sd, bsd = self.rot("t32")
        P.act(M("activation", out=sd[:], in_=sps[:], func=AF.Sqrt, scale=1.0 / (DM * hw * hw), bias=EPS / (hw * hw)),
              reads=[bsps], writes=[bsd])
        rs, brs = self.rot("rs")
        P.dve(M("reciprocal", out=rs[:], in_=sd[:]), reads=[bsd], writes=[brs])
        return rs, brs

    def prenorm_tt(self, l, w, tt):
        P = self.P
        ph = P.phase
        P.phase = "pre"
        tsl = slice(tt * TT, (tt + 1) * TT)
        sps, bsps = self.ps(hold=True)
        for c in range(8):
            sq, bsq = self.rot("tb")
            P.act(M("activation", out=sq[:], in_=self.X[:, c, tsl], func=AF.Square), reads=[self.bX[c][tt]], writes=[bsq])
            P.pe(M("matmul", sps[:], lhsT=self.cb[:, 1, :], rhs=sq[:], start=(c == 0), stop=(c == 7)),
                 reads=[bsq, self.bcb], writes=[bsps], accum=(c > 0))
        rs, brs = self.rstd_from(sps, bsps, 1.0)
        self.ps_release(sps)
        for c in range(8):
            g = self.pcol(("pre", l, w), c)
            t, bt = self.rot("t32")
            P.act(M("activation", out=t[:], in_=self.X[:, c, tsl], func=AF.Copy, scale=g), reads=[self.bX[c][tt], self.bpv], writes=[bt])
            P.dve(M("tensor_tensor", out=self.H[:, c, tsl], in0=t[:], in1=rs[:], op=ALU.mult), reads=[bt, brs], writes=[self.bH[c][tt]])
        P.phase = ph

    def prenorm(self, l, w):
        for tt in range(2):
            self.prenorm_tt(l, w, tt)

    def finish(self, l, w, spss, hw):
        for tt in range(2):
            self.postnorm(l, w, tt, spss[tt][0], spss[tt][1], hw)
            if self.next_norm is not None:
                self.prenorm_tt(self.next_norm[0], self.next_norm[1], tt)

    def ybuf(self, tt, c):
        if tt == 0:
            return self.Ysb[:, c * TT:(c + 1) * TT], [self.bH[c][0], self.bH[c][1]]
        return self.Ysb2[:, c * TT:(c + 1) * TT], [self.bY2[c]]

    def evac_y(self, py, bpy, c, tt, sps, bsps):
        P = self.P
        yv, yb = self.ybuf(tt, c)
        P.dve(M("tensor_copy", out=yv, in_=py[:]), reads=[bpy], writes=yb)
        sq, bsq = self.rot("tb")
        P.act(M("activation", out=sq[:], in_=yv, func=AF.Square), reads=yb, writes=[bsq])
        self.pending_stats.append((M("matmul", sps[:], lhsT=self.cb[:, 1, :], rhs=sq[:], start=(c == 0), stop=(c == 7)),
                                   [bsq, self.bcb], [bsps], c > 0))

    def flush_stats(self, keep=0):
        while len(self.pending_stats) > keep:
            fn, rd, wr, acc = self.pending_stats.pop(0)
            self.P.pe(fn, reads=rd, writes=wr, accum=acc)

    def postnorm(self, l, w, tt, sps, bsps, hw):
        P = self.P
        tsl = slice(tt * TT, (tt + 1) * TT)
        rs, brs = self.rstd_from(sps, bsps, hw)
        self.ps_release(sps)
        for c in range(8):
            g = self.pcol(("post", l, w), c)
            t, bt = self.rot("t32")
            yv, yb = self.ybuf(tt, c)
            P.act(M("activation", out=t[:], in_=yv, func=AF.Copy, scale=g), reads=yb + [self.bpv], writes=[bt])
            P.dve(M("tensor_tensor", out=t[:], in0=t[:], in1=rs[:], op=ALU.mult), reads=[bt, brs], writes=[bt])
            P.dve(M("tensor_tensor", out=self.X[:, c, tsl], in0=self.X[:, c, tsl], in1=t[:], op=ALU.add),
                  reads=[self.bX[c][tt], bt], writes=[self.bX[c][tt]])

    def make_ysb2(self, byte_off):
        self.Ysb2 = self.arena_view(byte_off, [128, 8 * TT], F32)
        haz = {}
        dmas = []
        for b in self.arena_bufs:
            ops = list(b.r)
            if b.w is not None:
                ops.append(b.w)
            for op in ops:
                if op.is_dma:
                    dmas.append(op)
                else:
                    cur = haz.get(op.eng)
                    if cur is None or op.idx > cur.idx:
                        haz[op.eng] = op
        hz = list(haz.values()) + dmas + list(self.arena_haz)
        self.bY2 = []
        for c in range(8):
            b = Buf("Y2_%d" % c)
            b.r = list(hz)
            self.arena_bufs.append(b)
            self.bY2.append(b)

    def ffn(self, l, f, half):
        P = self.P
        w = 0 if f == 0 else 2
        self.ps_set = list(range(8))
        P.phase = "ffnP1"
        self.arena_reset()
        A = self.arena_view(0, [128, NJ, SEG], BF16).rearrange("p (j t) -> p j t", j=NJ)
        bA = [[self.abuf("A%d_%d" % (j, t)) for t in range(2)] for j in range(NJ)]
        for jp in range(NJ // 2):
            wvs = []
            for j in (2 * jp, 2 * jp + 1):
                wt, bw = self.wload(("gu", l, f, j))
                wvs.append((j, wt[:, 0:2048].rearrange("p (s c f) -> p s c f", s=2, c=8), bw))
            for tt in range(2):
                tsl = slice(tt * TT, (tt + 1) * TT)
                for j, wv, bw in wvs:
                    pg, bg = self.ps()
                    pu, bu = self.ps()
                    for s_, (pp, bp) in enumerate(((pg, bg), (pu, bu))):
                        for c in range(8):
                            P.pe(M("matmul", pp[:], lhsT=wv[:, s_, c, :], rhs=self.H[:, c, tsl], start=(c == 0), stop=(c == 7)),
                                 reads=[bw, self.bH[c][tt]], writes=[bp], accum=(c > 0))
                    sg, bsg = self.rot("t32")
                    P.act(M("activation", out=sg[:], in_=pg[:], func=AF.Silu), reads=[bg], writes=[bsg])
                    P.dve(M("tensor_tensor", out=A[:, j, tsl], in0=sg[:], in1=pu[:], op=ALU.mult),
                          reads=[bsg, bu], writes=[bA[j][tt]])
        if self.stop_after == "phase1":
            return
        P.phase = "ffnP2"
        self.make_ysb2(NJ * SEG * 2)
        spss = [self.ps(hold=True) for _ in range(2)]
        order = [(c, tt) for c in range(6) for tt in range(2)] + [(6, 0), (7, 0), (6, 1), (7, 1)]
        wmap = {}
        for c, tt in order:
            if c not in wmap:
                wt, bw = self.wload(("d", l, f, c))
                wmap[c] = (wt[:, 0:2816].rearrange("p (j f) -> p j f", j=NJ), bw)
            wv, bw = wmap[c]
            tsl = slice(tt * TT, (tt + 1) * TT)
            py, bpy = self.ps()
            for j in range(NJ):
                P.pe(M("matmul", py[:], lhsT=wv[:, j, :], rhs=A[:, j, tsl], start=(j == 0), stop=(j == NJ - 1)),
                     reads=[bw, bA[j][tt]], writes=[bpy], accum=(j > 0))
            self.flush_stats()
            self.evac_y(py, bpy, c, tt, spss[tt][0], spss[tt][1])
        self.flush_stats()
        self.finish(l, w, spss, 0.5)

    def out_proj(self, l, CAT, bCAT):
        P = self.P
        P.phase = "oproj"
        self.make_ysb2(16384)
        spss = [self.ps(hold=True) for _ in range(2)]
        order = [(cc, tt) for cc in range(6) for tt in range(2)] + [(6, 0), (7, 0), (6, 1), (7, 1)]
        wmap = {}
        for cc, tt in order:
            g, s_ = cc // 2, cc % 2
            if g not in wmap:
                wt, bw = self.wload(("out", l, g))
                wmap[g] = (wt[:, 0:2048].rearrange("p (s c f) -> p s c f", s=2, c=8), bw)
            wv, bw = wmap[g]
            tsl = slice(tt * TT, (tt + 1) * TT)
            py, bpy = self.ps()
            for c in range(8):
                P.pe(M("matmul", py[:], lhsT=wv[:, s_, c, :], rhs=CAT[:, c, tsl], start=(c == 0), stop=(c == 7)),
                     reads=[bw, bCAT[c][tt]], writes=[bpy], accum=(c > 0))
            self.flush_stats()
            self.evac_y(py, bpy, cc, tt, spss[tt][0], spss[tt][1])
        self.flush_stats()
        self.finish(l, 1, spss, 1.0)

    def mixer_odd(self, l, half):
        P = self.P
        o = l // 2
        self.ps_set = list(range(8))
        P.phase = "odd"
        self.arena_reset()
        CAT = self.arena_view(0, [128, 8, SEG], BF16).rearrange("p (c t) -> p c t", c=8)
        bCAT = [[self.abuf("CAT%d_%d" % (c, t)) for t in range(2)] for c in range(8)]
        Z = [self.arena_view(16384 + i * 2064, [128, 516], F32) for i in range(2)]
        bZ = [self.abuf("Z%d" % i) for i in range(2)]
        zi = 0
        for c in range(8):
            wt, bw = self.wload(("cin", o, c))
            wv = wt[:, 0:3072].rearrange("p (s c f) -> p s c f", s=3, c=8)
            for tt in range(2):
                tsl = slice(tt * TT, (tt + 1) * TT)
                pps = [self.ps() for _ in range(3)]
                for s_ in range(3):
                    pp, bp = pps[s_]
                    for k in range(8):
                        P.pe(M("matmul",
                            pp[:], lhsT=wv[:, s_, k, :], rhs=self.H[:, k, tsl], start=(k == 0), stop=(k == 7)),
                            reads=[bw, self.bH[k][tt]], writes=[bp], accum=(k > 0))
                (pc, bpc), (px, bpx), (pb, bpb) = pps
                z, bz = Z[zi % 2], bZ[zi % 2]
                zi += 1
                zc, bzc = self.rot("t32")
                P.act(M("activation", out=zc[:], in_=pc[:], func=AF.Copy), reads=[bpc], writes=[bzc])
                P.dve(M("tensor_copy", out=z[:, 0:2], in_=self.zh[:, o, c, :]), reads=[self.bzh[o][c]], writes=[bz])
                P.dve(M("tensor_tensor", out=z[:, 2:514], in0=zc[:], in1=px[:], op=ALU.mult),
                      reads=[bzc, bpx, bz], writes=[bz])
                P.dve(M("tensor_copy", out=self.zh[:, o, c, :], in_=z[:, 512:514]), reads=[bz], writes=[self.bzh[o][c]])
                y, by = self.rot("t32")
                P.dve(M("tensor_scalar", out=y[:], in0=z[:, 0:512], scalar1=self.pcol(("ccw", o, 0), c),
                                                           scalar2=None, op0=ALU.mult), reads=[bz, self.bpv], writes=[by])
                for j in (1, 2):
                    P.dve(M("scalar_tensor_tensor",
                        out=y[:], in0=z[:, j:j + 512], scalar=self.pcol(("ccw", o, j), c), in1=y[:], op0=ALU.mult, op1=ALU.add),
                        reads=[bz, by, self.bpv], writes=[by])
                P.dve(M("tensor_tensor", out=CAT[:, c, tsl], in0=y[:], in1=pb[:], op=ALU.mult),
                      reads=[by, bpb], writes=[bCAT[c][tt]])
        self.out_proj(l, CAT, bCAT)

    def mixer_even(self, l, half):
        P = self.P
        e_ = l // 2
        self.ps_set = list(range(8))
        P.phase = "lru"
        self.arena_reset()
        off = [0]

        def carve(shape, dt):
            n = int(np.prod(shape[1:])) * (2 if dt == BF16 else 4)
            v = self.arena_view(off[0], shape, dt)
            off[0] += (n + 3) // 4 * 4
            return v
        CAT = carve([128, 8, SEG], BF16).rearrange("p (c t) -> p c t", c=8)
        bCAT = [[self.abuf("CAT%d_%d" % (c, t)) for t in range(2)] for c in range(8)]
        QT = carve([128, 4, SEG], BF16).rearrange("p (c t) -> p c t", c=4)
        bQT = [[self.abuf("QT%d_%d" % (c, t)) for t in range(2)] for c in range(4)]
        ropet = carve([128, 2, SEG], F32).rearrange("p (a t) -> p a t", a=2)
        brope = self.abuf("rope")
        if half == 0:
            KTc, bKTc = self.KTs[e_], self.bKTs[e_]
            Vc, bVc = self.Vs[e_], self.bVs[e_]
        else:
            KTc = carve([128, 4, SEG], BF16).rearrange("p (c t) -> p c t", c=4)
            bKTc = [self.abuf("KTc%d" % c) for c in range(4)]
            Vc = carve([128, 8, 520], BF16).rearrange("p (k d) -> p k d", k=8)
            bVc = [self.abuf("Vc%d" % k) for k in range(8)]
            v4 = Vc.rearrange("p k (h d) -> p k h d", h=8)
            P.dve(M("memset", v4[:, :, :, 64:65], 1.0), writes=bVc)
        XR = carve([128, 516], F32)
        bXR = self.abuf("XR")
        lt = [carve([128, TT], F32) for _ in range(6)]
        blt = [self.abuf("lt%d" % i) for i in range(6)]
        xcb = carve([128, TT], BF16)
        bxcb = self.abuf("xcb")
        PT = [carve([128, 256], BF16) for _ in range(8)]
        bPT = [self.abuf("PT%d" % i) for i in range(8)]
        atok = [carve([128, 512], F32) for _ in range(2)]
        batok = [self.abuf("atok%d" % i) for i in range(2)]
        acc = [carve([128, 68], F32) for _ in range(2)]
        bacc = [self.abuf("acc%d" % i) for i in range(2)]
        KMb = carve([128, 4, 8], BF16).rearrange("p (c n) -> p c n", c=4)
        bKM = self.abuf("KM")
        KMf = carve([128, 4, 8], F32).rearrange("p (c n) -> p c n", c=4)
        bKMf = self.abuf("KMf")
        g8 = [carve([128, 8], F32) for _ in range(4)]
        bg8 = [self.abuf("g8_%d" % i) for i in range(4)]
        top8 = [carve([128, 8], F32) for _ in range(4)]
        btop8 = [self.abuf("top8_%d" % i) for i in range(4)]
        sel = [carve([128, 8], F32) for _ in range(4)]
        bsel = [self.abuf("sel%d" % i) for i in range(4)]
        qm = carve([128, 4, 2], F32).rearrange("p (c t) -> p c t", c=4)
        bqm = self.abuf("qm")
        km = carve([128, 4, 2], F32).rearrange("p (c t) -> p c t", c=4)
        bkm = self.abuf("km")
        negM = carve([128, 4], F32)
        bnegM = self.abuf("negM")
        sm1 = carve([128, 4], F32)
        sm2 = carve([128, 4], F32)
        bsm = self.abuf("sm")
        rcp = [carve([128, 2], F32) for _ in range(2)]
        brcp = [self.abuf("rcp%d" % i) for i in range(2)]

        pos0 = half * SEG
        P.dma("sp", M("dma_start", out=ropet, in_=self.rope_d.rearrange("p (a t) -> p a t", a=2)[:, :, pos0:pos0 + SEG]),
              writes=[brope])

        for c in range(4):
            wt, bw = self.wload(("lru", e_, c))
            wv = wt[:, 0:2048].rearrange("p (s c f) -> p s c f", s=2, c=8)
            for tt in range(2):
                tsl = slice(tt * TT, (tt + 1) * TT)
                pgg, bpg = self.ps()
                pxx, bpx = self.ps()
                for s_, (pp, bp) in enumerate(((pgg, bpg), (pxx, bpx))):
                    for k in range(8):
                        P.pe(M("matmul",
                            pp[:], lhsT=wv[:, s_, k, :], rhs=self.H[:, k, tsl], start=(k == 0), stop=(k == 7)),
                            reads=[bw, self.bH[k][tt]], writes=[bp], accum=(k > 0))
                gg, xc, r_, i_, a_, s2 = lt
                bgg, bxc, br, bi, ba, bs2 = blt
                P.act(M("activation", out=gg[:], in_=pgg[:], func=AF.Gelu_apprx_tanh), reads=[bpg], writes=[bgg])
                P.dve(M("tensor_copy", out=XR[:, 0:3], in_=self.lh[:, e_, c, 0:3]), reads=[self.blh[e_][c]], writes=[bXR])
                P.act(M("activation", out=XR[:, 3:515], in_=pxx[:], func=AF.Copy), reads=[bpx, bXR], writes=[bXR])
                P.dve(M("tensor_copy", out=self.lh[:, e_, c, 0:3], in_=XR[:, 512:515]), reads=[bXR], writes=[self.blh[e_][c]])
                P.dve(M("tensor_scalar", out=xc[:], in0=XR[:, 0:512], scalar1=self.pcol(("lcw", e_, 0), c),
                                                     scalar2=self.pcol(("lcb", e_), c), op0=ALU.mult, op1=ALU.add),
                      reads=[bXR, self.bpv], writes=[bxc])
                for j in (1, 2, 3):
                    P.dve(M("scalar_tensor_tensor",
                        out=xc[:], in0=XR[:, j:j + 512], scalar=self.pcol(("lcw", e_, j), c), in1=xc[:], op0=ALU.mult, op1=ALU.add),
                        reads=[bXR, bxc, self.bpv], writes=[bxc])
                P.act(M("activation", out=xcb[:], in_=xc[:], func=AF.Copy), reads=[bxc], writes=[bxcb])
                pr, bpr = self.ps()
                pi, bpi = self.ps()
                P.pe(M("matmul", pr[:], lhsT=self.wsm[:, (e_ * 2 + 0) * 4 + c, :], rhs=xcb[:], start=True, stop=True),
                     reads=[bxcb, self.bwsm], writes=[bpr])
                P.pe(M("matmul", pi[:], lhsT=self.wsm[:, (e_ * 2 + 1) * 4 + c, :], rhs=xcb[:], start=True, stop=True),
                     reads=[bxcb, self.bwsm], writes=[bpi])
                P.act(M("activation", out=r_[:], in_=pr[:], func=AF.Sigmoid, bias=self.pcol(("ba", e_), c)),
                      reads=[bpr, self.bpv], writes=[br])
                P.act(M("activation", out=i_[:], in_=pi[:], func=AF.Sigmoid, bias=self.pcol(("bx", e_), c)),
                      reads=[bpi, self.bpv], writes=[bi])
                P.act(M("activation", out=a_[:], in_=r_[:], func=AF.Exp, scale=self.lc[:, e_, c, 0:1]),
                      reads=[br, self.blc], writes=[ba])
                P.act(M("activation", out=s2[:], in_=r_[:], func=AF.Exp, scale=self.lc[:, e_, c, 1:2]),
                      reads=[br, self.blc], writes=[bs2])
                P.act(M("activation", out=s2[:], in_=s2[:], func=AF.Sqrt, scale=-1.0, bias=1.0), reads=[bs2], writes=[bs2])
                P.dve(M("tensor_tensor", out=i_[:], in0=i_[:], in1=xc[:], op=ALU.mult), reads=[bi, bxc], writes=[bi])
                P.dve(M("tensor_tensor", out=i_[:], in0=i_[:], in1=s2[:], op=ALU.mult), reads=[bi, bs2], writes=[bi])
                P.dve(M("tensor_tensor_scan", out=r_[:], data0=a_[:], data1=i_[:], initial=self.hst[:, e_, c:c + 1],
                                                          op0=ALU.mult, op1=ALU.add),
                      reads=[ba, bi, self.bhst[e_][c], br], writes=[br])
                P.dve(M("tensor_copy", out=self.hst[:, e_, c:c + 1], in_=r_[:, TT - 1:TT]), reads=[br], writes=[self.bhst[e_][c]])
                P.dve(M("tensor_tensor", out=CAT[:, 4 + c, tsl], in0=r_[:], in1=gg[:], op=ALU.mult),
                      reads=[br, bgg], writes=[bCAT[4 + c][tt]])

        P.phase = "qk"
        for c in range(4):
            wt, bw = self.wload(("qk", e_, c))
            wv = wt[:, 0:2048].rearrange("p (s c f) -> p s c f", s=2, c=8)
            for tt in range(2):
                tsl = slice(tt * TT, (tt + 1) * TT)
                for s_ in range(2):
                    pq, bpq = self.ps()
                    for k in range(8):
                        P.pe(M("matmul",
                            pq[:], lhsT=wv[:, s_, k, :], rhs=self.H[:, k, tsl], start=(k == 0), stop=(k == 7)),
                            reads=[bw, self.bH[k][tt]], writes=[bpq], accum=(k > 0))
                    qraw, bqraw = self.rot("tb")
                    P.act(M("activation", out=qraw[:], in_=pq[:], func=AF.Copy), reads=[bpq], writes=[bqraw])
                    prot, bprot = self.ps()
                    P.pe(M("matmul", prot[:], lhsT=self.cb[:, 2, :], rhs=qraw[:], start=True, stop=True),
                         reads=[bqraw, self.bcb], writes=[bprot])
                    t1, bt1 = self.rot("t32")
                    t2, bt2 = self.rot("t32")
                    P.dve(M("tensor_tensor", out=t1[:], in0=pq[:], in1=ropet[:, 0, tsl], op=ALU.mult),
                          reads=[bpq, brope, bqraw], writes=[bt1])
                    P.dve(M("tensor_tensor", out=t2[:], in0=prot[:], in1=ropet[:, 1, tsl], op=ALU.mult),
                          reads=[bprot, brope], writes=[bt2])
                    if s_ == 0:
                        dst, bd = QT[:, c, tsl], bQT[c][tt]
                    else:
                        dst, bd = KTc[:, c, tsl], bKTc[c]
                    P.dve(M("tensor_tensor", out=dst, in0=t1[:], in1=t2[:], op=ALU.add),
                          reads=[bt1, bt2], writes=[bd])

        P.phase = "v"
        for vh in range(2):
            wt, bw = self.wload(("v", e_, vh))
            wv = wt[:, 0:2048].rearrange("p (c f) -> p c f", c=8)
            for tk in range(8):
                pv_, bpv_ = self.ps()
                for k in range(8):
                    P.pe(M("matmul",
                        pv_[:, 0:256], lhsT=self.H[:, k, tk * 128:(tk + 1) * 128], rhs=wv[:, k, :], start=(k == 0), stop=(k == 7)),
                        reads=[bw, self.bH[k][tk // 4]], writes=[bpv_], accum=(k > 0))
                dst = Vc[:, tk, :].rearrange("p (h d) -> p h d", h=8)[:, vh * 4:(vh + 1) * 4, 0:64]
                P.act(M("activation", out=dst, in_=pv_[:, 0:256].rearrange("p (h d) -> p h d", h=4), func=AF.Copy),
                      reads=[bpv_], writes=[bVc[tk]])

        P.phase = "attn"
        scale = 0.125
        for c in range(4):
            for which, (src, bsrc, dstm, bdm) in enumerate(((QT, None, qm, bqm), (KTc, None, km, bkm))):
                for tt in range(2):
                    tsl = slice(tt * TT, (tt + 1) * TT)
                    rb = [bQT[c][tt]] if which == 0 else [bKTc[c]]
                    sq, bsq = self.rot("tb")
                    P.act(M("activation", out=sq[:], in_=src[:, c, tsl], func=AF.Square),
                          reads=rb, writes=[bsq])
                    pn, bpn = self.ps()
                    P.pe(M("matmul", pn[:], lhsT=self.cb[:, 1, :], rhs=sq[:], start=True, stop=True),
                         reads=[bsq, self.bcb], writes=[bpn])
                    P.dve(M("tensor_reduce", out=dstm[:, c, tt:tt + 1], in_=pn[:], axis=AX.X, op=ALU.max),
                          reads=[bpn], writes=[bdm])
        P.dve(M("tensor_tensor", out=sm1[:], in0=qm[:, :, 0], in1=qm[:, :, 1], op=ALU.max), reads=[bqm], writes=[bsm])
        P.dve(M("tensor_tensor", out=sm2[:], in0=km[:, :, 0], in1=km[:, :, 1], op=ALU.max), reads=[bkm, bsm], writes=[bsm])
        if half == 0:
            P.dve(M("tensor_copy", out=self.kmx[:, e_, :], in_=sm2[:]), reads=[bsm], writes=[self.bkmx[e_]])
        else:
            P.dve(M("tensor_tensor", out=sm2[:], in0=sm2[:], in1=self.kmx[:, e_, :], op=ALU.max),
                  reads=[bsm, self.bkmx[e_]], writes=[bsm])
        P.dve(M("tensor_tensor", out=sm1[:], in0=sm1[:], in1=sm2[:], op=ALU.mult), reads=[bsm], writes=[bsm])
        P.act(M("activation", out=sm1[:], in_=sm1[:], func=AF.Sqrt), reads=[bsm], writes=[bsm])
        P.dve(M("tensor_scalar", out=negM[:], in0=sm1[:], scalar1=-scale, scalar2=None, op0=ALU.mult), reads=[bsm], writes=[bnegM])

        def kt_src(kc):
            if kc < 8 and half == 1:
                return self.KTs[e_], self.bKTs[e_], kc
            return KTc, bKTc, kc % 8

        def v_src(kc):
            if kc < 8 and half == 1:
                return self.Vs[e_], self.bVs[e_], kc
            return Vc, bVc, kc % 8

        if half == 1:
            for c in range(4):
                P.dve(M("tensor_reduce", out=KMf[:, c, 0:4], in_=self.KTs[e_][:, c, :].rearrange("p (n k) -> p n k", n=4),
                                                     axis=AX.X, op=ALU.add), reads=[self.bKTs[e_][c]], writes=[bKMf])
                P.dve(M("tensor_reduce", out=KMf[:, c, 4:8], in_=KTc[:, c, :].rearrange("p (n k) -> p n k", n=4),
                                                     axis=AX.X, op=ALU.add), reads=[bKTc[c]], writes=[bKMf])
            P.dve(M("tensor_scalar", out=KMb[:], in0=KMf[:], scalar1=1.0 / 256, scalar2=None, op0=ALU.mult),
                  reads=[bKMf], writes=[bKM])

        self.ps_set = [4, 5, 6, 7]
        OB = [self.PS[i] for i in range(4)]
        bOB = [self.bPS[i] for i in range(4)]
        pti = [0]
        LAG = 2
        for qg in range(4):
            own = 4 * half + qg
            q0 = qg * 256
            nkc = 2 * own + 2
            use_sel = own >= 4

            def emit_sel(hd):
                c = hd // 2
                hp = 64 * (hd % 2)
                par = hd % 2
                for qt in range(2):
                    k_ = qt * 2 + par
                    pgt, bpgt = self.ps()
                    P.pe(M("matmul", pgt[:, 0:8], lhsT=QT[hp:hp + 64, c, q0 + qt * 128:q0 + (qt + 1) * 128],
                           rhs=KMb[hp:hp + 64, c, :], start=True, stop=True), reads=[bQT[c][qg // 2], bKM], writes=[bpgt])
                    P.dve(M("memset", g8[k_][:], -1e30), writes=[bg8[k_]])
                    P.dve(M("tensor_copy", out=g8[k_][:, 0:own], in_=pgt[:, 0:own]), reads=[bpgt, bg8[k_]], writes=[bg8[k_]])
                    P.dve(M("max", out=top8[k_][:], in_=g8[k_][:]), reads=[bg8[k_]], writes=[btop8[k_]])
                    P.dve(M("tensor_scalar", out=sel[k_][:], in0=g8[k_][:], scalar1=top8[k_][:, 2:3], scalar2=None, op0=ALU.is_ge),
                          reads=[bg8[k_], btop8[k_]], writes=[bsel[k_]])

            def emit_S(hd, kc):
                c = hd // 2
                hp = 64 * (hd % 2)
                ktt, bkt, kl = kt_src(kc)
                dq = kc - 2 * own
                qlo = 128 if dq == 1 else 0
                pS, bpS = self.ps(hold=True)
                P.pe(M("matmul", pS[:, qlo:256], lhsT=ktt[hp:hp + 64, c, kl * 128:(kl + 1) * 128],
                       rhs=QT[hp:hp + 64, c, q0 + qlo:q0 + 256], start=True, stop=True),
                     reads=[bkt[c], bQT[c][qg // 2]], writes=[bpS])
                return pS, bpS, qlo, dq

            def emit_EV(hd, kc, st_):
                pS, bpS, qlo, dq = st_
                c = hd // 2
                vt, bv, vl = v_src(kc)
                n = kc // 2
                pt_, bpt_ = PT[pti[0] % 8], bPT[pti[0] % 8]
                pti[0] += 1
                P.act(M("activation", out=pt_[:, qlo:256], in_=pS[:, qlo:256], func=AF.Exp, scale=scale, bias=negM[:, c:c + 1]),
                      reads=[bpS, bnegM], writes=[bpt_])
                self.ps_release(pS)
                if dq >= 0:
                    P.dve(M("tensor_tensor", out=pt_[:, qlo:qlo + 128], in0=pt_[:, qlo:qlo + 128], in1=self.cb[:, 3, :], op=ALU.mult),
                          reads=[bpt_, self.bcb], writes=[bpt_])
                for qt in range(2):
                    if qt * 128 < qlo:
                        continue
                    gq = 2 * own + qt
                    if use_sel:
                        reg = n
                        first = (kc % 2 == 0)
                        last = (kc % 2 == 1) or (kc == gq)
                        bank = qt * 2 + reg // 4
                    else:
                        reg = 0
                        first = (kc == 0)
                        last = (kc == gq)
                        bank = qt * 2 + hd % 2
                    ob, bob = OB[bank], bOB[bank]
                    P.pe(M("matmul", ob[:, (reg % 4) * 65:(reg % 4) * 65 + 65], lhsT=pt_[:, qt * 128:(qt + 1) * 128],
                           rhs=vt[:, vl, hd * 65:(hd + 1) * 65], start=first, stop=last),
                         reads=[bpt_, bv[vl]], writes=[bob], accum=(not first))

            def emit_combine(hd):
                par = hd % 2
                for qt in range(2):
                    ac, bac = acc[qt], bacc[qt]
                    if use_sel:
                        k_ = qt * 2 + par
                        ob, bob = OB[qt * 2 + own // 4], bOB[qt * 2 + own // 4]
                        P.dve(M("tensor_copy", out=ac[:, 0:65], in_=ob[:, (own % 4) * 65:(own % 4) * 65 + 65]), reads=[bob], writes=[bac])
                        for n in range(own):
                            ob, bob = OB[qt * 2 + n // 4], bOB[qt * 2 + n // 4]
                            P.dve(M("scalar_tensor_tensor", out=ac[:, 0:65], in0=ob[:, (n % 4) * 65:(n % 4) * 65 + 65],
                                    scalar=sel[k_][:, n:n + 1], in1=ac[:, 0:65], op0=ALU.mult, op1=ALU.add),
                                  reads=[bob, bsel[k_], bac], writes=[bac])
                    else:
                        ob, bob = OB[qt * 2 + par], bOB[qt * 2 + par]
                        P.dve(M("tensor_copy", out=ac[:, 0:65], in_=ob[:, 0:65]), reads=[bob], writes=[bac])
                    P.dve(M("reciprocal", out=rcp[qt][:, 0:1], in_=ac[:, 64:65]), reads=[bac], writes=[brcp[qt]])
                    P.dve(M("tensor_scalar", out=atok[qt][:, hd * 64:(hd + 1) * 64], in0=ac[:, 0:64], scalar1=rcp[qt][:, 0:1],
                            scalar2=None, op0=ALU.mult), reads=[bac, brcp[qt]], writes=[batok[qt]])

            seq_ = [(hd, kc) for hd in range(8) for kc in range(nkc)]
            states = {}
            for i in range(len(seq_) + LAG):
                if i < len(seq_):
                    hd, kc = seq_[i]
                    if kc == 0 and use_sel:
                        emit_sel(hd)
                    states[i] = emit_S(hd, kc)
                j = i - LAG
                if j >= 0:
                    hd, kc = seq_[j]
                    emit_EV(hd, kc, states.pop(j))
                    if kc == nkc - 1:
                        emit_combine(hd)
            for qt in range(2):
                ptp, bptp = self.ps()
                for cc in range(4):
                    P.pe(M("transpose", out=ptp[:, cc * 128:(cc + 1) * 128],
                                                                      in_=atok[qt][:, cc * 128:(cc + 1) * 128], identity=self.identf[:]),
                         reads=[batok[qt], self.bident], writes=[bptp], accum=(cc > 0))
                col = q0 + qt * 128
                P.act(M("activation", out=CAT[:, 0:4, col:col + 128],
                                                               in_=ptp[:].rearrange("p (a b) -> p a b", a=4), func=AF.Copy),
                      reads=[bptp], writes=[bCAT[cc][qg // 2] for cc in range(4)])
        self.ps_set = list(range(8))
        if self.dbg:
            d = P.dma("pool", M("dma_start", out=self.dbg_d[half].rearrange("p (c t) -> p c t", c=8), in_=CAT),
                      reads=[b for bb in bCAT for b in bb])
            self.finals.append(d)
        self.out_proj(l, CAT, bCAT)


_CACHE = {}


def _get_prog(nseq, layers, nhalf):
    key = (nseq, tuple(layers), nhalf)
    if key not in _CACHE:
        _CACHE[key] = Builder(nseq, layers, nhalf).build()
    return _CACHE[key]


def kernel(**inputs):
    inp = {k: np.asarray(v) for k, v in inputs.items()}
    x = np.ascontiguousarray(inp["x"], dtype=np.float32)
    wall = pack_weights(inp)
    pv, wsm = pack_small(inp)
    cm, rope = const_tables()
    nc = _get_prog(2, (0, 1, 2, 3), 2)
    in_maps = []
    for i in range(NCORES):
        in_maps.append({"xs": x[2 * i:2 * i + 2], "wall": wall, "pv_in": pv, "wsm_in": wsm, "cm_in": cm, "rope_in": rope})
    res = run_bass_kernel_spmd(nc, in_maps, core_ids=list(range(NCORES)))
    out = np.concatenate([r["ys"] for r in res.results], axis=0)
    return out.astype(np.float32)
```

```python
import numpy as np
from contextlib import ExitStack
import concourse.bass as bass
import concourse.mybir as mybir
from concourse.bass_utils import run_bass_kernel_spmd

F32 = mybir.dt.float32
BF16 = mybir.dt.bfloat16
AF = mybir.ActivationFunctionType
ALU = mybir.AluOpType
AX = mybir.AxisListType

ENGS = ("pe", "act", "dve", "pool", "sp")
NCORES = 8
SEQ = 2048
DM = 1024
DFF = 2816
NJ = 22
SEG = 1024
TT = 512
EPS = 1e-6
ROPE_THETA = 500000.0
WSLOT = 3072
N_WSLOT = 4


def M(name, *a, **kw):
    return (name, a, kw)


class Buf:
    __slots__ = ("name", "w", "r")

    def __init__(self, name):
        self.name = name
        self.w = None
        self.r = []


class Op:
    __slots__ = ("eng", "fn", "deps", "mark", "semval", "is_dma", "dma_sem", "dma_val", "idx", "phase")


class Prog:
    def __init__(self, nc, per_q=10):
        self.nc = nc
        self.ops = {e: [] for e in ENGS}
        self.per_q = per_q
        self.dma_q = {"sp": 0, "act": 1, "pool": 2}
        self.n_dma_sems = per_q * 3
        self.dma_sem_next = {"sp": 0, "act": 0, "pool": 0}
        self.dma_counts = [0] * self.n_dma_sems
        self.all_ops = []
        self.annotate = getattr(Prog, 'annotate_default', False)
        self.phase = ""

    def _add(self, eng, fn, reads, writes, is_dma=False, accum=False):
        op = Op()
        op.eng = eng
        op.fn = fn
        op.mark = False
        op.semval = None
        op.is_dma = is_dma
        op.dma_sem = None
        op.dma_val = None
        op.idx = len(self.ops[eng])
        op.phase = getattr(self, "phase", "")
        deps = {}
        for b in reads:
            if b.w is not None:
                deps[id(b.w)] = b.w
        for b in writes:
            if b.w is not None:
                if not (accum and b.w.eng == "pe" and eng == "pe" and not b.w.is_dma):
                    deps[id(b.w)] = b.w
            for r in b.r:
                deps[id(r)] = r
        deps.pop(id(op), None)
        op.deps = [d for d in deps.values()
                   if not (d.eng == "pe" and eng == "pe" and not d.is_dma and not is_dma)]
        for b in reads:
            b.r.append(op)
        for b in writes:
            b.w = op
            b.r = []
        if is_dma:
            k = self.dma_sem_next[eng]
            self.dma_sem_next[eng] = (k + 1) % self.per_q
            s = self.dma_q[eng] * self.per_q + k
            self.dma_counts[s] += 16
            op.dma_sem = s
            op.dma_val = self.dma_counts[s]
        self.ops[eng].append(op)
        self.all_ops.append(op)
        return op

    def pe(self, fn, reads=(), writes=(), accum=False):
        return self._add("pe", fn, reads, writes, accum=accum)

    def act(self, fn, reads=(), writes=()):
        return self._add("act", fn, reads, writes)

    def dve(self, fn, reads=(), writes=()):
        return self._add("dve", fn, reads, writes)

    def pool(self, fn, reads=(), writes=()):
        return self._add("pool", fn, reads, writes)

    def dma(self, eng, fn, reads=(), writes=()):
        return self._add(eng, fn, reads, writes, is_dma=True)

    def emit(self, final_wait_ops=()):
        nc = self.nc
        for op in self.all_ops:
            for d in op.deps:
                if not d.is_dma:
                    d.mark = True
        for op in final_wait_ops:
            if not op.is_dma:
                op.mark = True
        cnt = {e: 0 for e in ENGS}
        for e in ENGS:
            for op in self.ops[e]:
                if op.mark and not op.is_dma:
                    cnt[e] += 1
                    op.semval = cnt[e]
        self.stats = {}
        with ExitStack() as st:
            esem = {e: st.enter_context(nc.semaphore("es_" + e)) for e in ENGS}
            dsem = [st.enter_context(nc.semaphore("ds%d" % i)) for i in range(self.n_dma_sems)]
            block = st.enter_context(nc.Block())

            def run(engname, engobj):
                known_e = {e: 0 for e in ENGS}
                known_d = [0] * self.n_dma_sems
                nwait = 0
                for op in self.ops[engname]:
                    need_e = {}
                    need_d = {}
                    for d in op.deps:
                        if d.is_dma:
                            if d.dma_val > known_d[d.dma_sem]:
                                if d.dma_val > need_d.get(d.dma_sem, 0):
                                    need_d[d.dma_sem] = d.dma_val
                        else:
                            if d.semval > known_e[d.eng]:
                                if d.semval > need_e.get(d.eng, 0):
                                    need_e[d.eng] = d.semval
                    if op.is_dma and op.dma_val - 16 > known_d[op.dma_sem]:
                        if op.dma_val - 16 > need_d.get(op.dma_sem, 0):
                            need_d[op.dma_sem] = op.dma_val - 16
                    for e, v in need_e.items():
                        engobj.wait_ge(esem[e], v)
                        known_e[e] = v
                        nwait += 1
                    for s, v in need_d.items():
                        engobj.wait_ge(dsem[s], v)
                        known_d[s] = v
                        nwait += 1
                    ins = getattr(engobj, op.fn[0])(*op.fn[1], **op.fn[2])
                    if self.annotate and op.phase:
                        ins.annotate(op.phase)
                    if op.is_dma:
                        ins.then_inc(dsem[op.dma_sem], 16)
                    elif op.mark:
                        ins.then_inc(esem[engname], 1)
                if engname == "sp":
                    for op in final_wait_ops:
                        if op.is_dma:
                            if op.dma_val > known_d[op.dma_sem]:
                                engobj.wait_ge(dsem[op.dma_sem], op.dma_val)
                                known_d[op.dma_sem] = op.dma_val
                        else:
                            if op.semval > known_e[op.eng]:
                                engobj.wait_ge(esem[op.eng], op.semval)
                                known_e[op.eng] = op.semval
                self.stats[engname] = (len(self.ops[engname]), nwait)

            @block.tensor
            def _(eng):
                run("pe", eng)

            @block.scalar
            def _(eng):
                run("act", eng)

            @block.vector
            def _(eng):
                run("dve", eng)

            @block.gpsimd
            def _(eng):
                run("pool", eng)

            @block.sync
            def _(eng):
                run("sp", eng)


def wlayout():
    off = 0
    L = {}

    def add(key, n):
        nonlocal off
        L[key] = (off, n)
        off += n
    for l in range(4):
        for f in range(2):
            for j in range(NJ):
                add(("gu", l, f, j), 2048)
            for c in range(8):
                add(("d", l, f, c), 2816)
        if l % 2 == 0:
            e = l // 2
            for c in range(4):
                add(("lru", e, c), 2048)
            for c in range(4):
                add(("qk", e, c), 2048)
            for vh in range(2):
                add(("v", e, vh), 2048)
        else:
            o = l // 2
            for c in range(8):
                add(("cin", o, c), 3072)
        for g in range(4):
            add(("out", l, g), 2048)
    return L, off


def playout():
    off = 0
    L = {}

    def add(key, n=1):
        nonlocal off
        L[key] = off
        off += n
    for l in range(4):
        for w in range(3):
            add(("pre", l, w), 8)
            add(("post", l, w), 8)
    for e in range(2):
        for j in range(4):
            add(("lcw", e, j), 4)
        add(("lcb", e), 4)
        add(("ba", e), 4)
        add(("bx", e), 4)
        add(("lam", e), 4)
    for o in range(2):
        for j in range(3):
            add(("ccw", o, j), 8)
    return L, off


def _chunkT(W, col):
    return W[:, col:col + 128].reshape(8, 128, 128).transpose(1, 0, 2)


def pack_weights(inp):
    L, tot = wlayout()
    wall = np.empty((128, tot), np.float32)

    def put(key, arr):
        o, n = L[key]
        wall[:, o:o + n] = arr.reshape(128, n)
    for l in range(4):
        for f in range(2):
            if f == 0:
                wg, wu, wd = inp["ffn1_w_gate"][l], inp["ffn1_w_up"][l], inp["ffn1_w_down"][l]
            else:
                wg, wu, wd = inp["ffn2_w_gate"][l], inp["ffn2_w_up"][l], inp["ffn2_w_down"][l]
            for j in range(NJ):
                put(("gu", l, f, j), np.stack([_chunkT(wg, j * 128), _chunkT(wu, j * 128)], axis=1))
            for c in range(8):
                put(("d", l, f, c), wd[:, c * 128:(c + 1) * 128].reshape(NJ, 128, 128).transpose(1, 0, 2))
        if l % 2 == 0:
            e = l // 2
            win = inp["ab_w_in"][e]
            wout = inp["ab_w_out"][e]
            for c in range(4):
                put(("lru", e, c), np.stack([_chunkT(win, 2048 + c * 128), _chunkT(win, 1536 + c * 128)], axis=1))
                put(("qk", e, c), np.stack([_chunkT(win, c * 128), _chunkT(win, 512 + c * 128)], axis=1))
            for vh in range(2):
                put(("v", e, vh), win[:, 1024 + vh * 256:1024 + (vh + 1) * 256].reshape(8, 128, 256).transpose(1, 0, 2))
        else:
            o = l // 2
            win = inp["c_w_in"][o]
            wout = inp["c_w_out"][o]
            for c in range(8):
                put(("cin", o, c), np.stack([_chunkT(win, 1024 + c * 128), _chunkT(win, 2048 + c * 128),
                                             _chunkT(win, c * 128)], axis=1))
        for g in range(4):
            put(("out", l, g), np.stack([_chunkT(wout, (2 * g) * 128), _chunkT(wout, (2 * g + 1) * 128)], axis=1))
    return wall


def pack_small(inp):
    L, n = playout()
    pv = np.zeros((128, n), np.float32)

    def colmajor(v):
        return v.reshape(-1, 128).T
    for l in range(4):
        for w in range(3):
            pv[:, L[("pre", l, w)]:L[("pre", l, w)] + 8] = colmajor(inp["norm_pre"][l, w])
            pv[:, L[("post", l, w)]:L[("post", l, w)] + 8] = colmajor(inp["norm_post"][l, w])
    for e in range(2):
        for j in range(4):
            pv[:, L[("lcw", e, j)]:L[("lcw", e, j)] + 4] = colmajor(inp["lru_conv_w"][e, j])
        pv[:, L[("lcb", e)]:L[("lcb", e)] + 4] = colmajor(inp["lru_conv_b"][e])
        pv[:, L[("ba", e)]:L[("ba", e)] + 4] = colmajor(inp["lru_gate_a_b"][e].reshape(-1))
        pv[:, L[("bx", e)]:L[("bx", e)] + 4] = colmajor(inp["lru_gate_x_b"][e].reshape(-1))
        pv[:, L[("lam", e)]:L[("lam", e)] + 4] = colmajor(inp["lru_lambda"][e])
    for o in range(2):
        for j in range(3):
            pv[:, L[("ccw", o, j)]:L[("ccw", o, j)] + 8] = colmajor(inp["c_conv_w"][o, j])
    wsm = np.zeros((128, 16, 128), np.float32)
    for e in range(2):
        for gi, nm in enumerate(("lru_gate_a_w", "lru_gate_x_w")):
            w = inp[nm][e]
            for c in range(4):
                idx = (e * 2 + gi) * 4 + c
                wsm[0:64, idx, 0:64] = w[2 * c]
                wsm[64:128, idx, 64:128] = w[2 * c + 1]
    return pv, wsm.reshape(128, 16 * 128)


def const_tables():
    cm = np.zeros((128, 4, 128), np.float32)
    cm[:, 0, :] = np.eye(128, dtype=np.float32)
    cm[:, 1, :] = 1.0
    for m in range(128):
        j = m % 64
        if j < 8:
            cm[m + 8, 2, m] = 1.0
        elif j < 16:
            cm[m - 8, 2, m] = 1.0
    k = np.arange(128)
    cm[:, 3, :] = (k[:, None] <= k[None, :]).astype(np.float32)
    inv_freq = (np.float32(ROPE_THETA) ** (-np.arange(0, 16, 2, dtype=np.float32) / np.float32(16))).astype(np.float32)
    pos = np.arange(SEQ, dtype=np.float32)
    ang = (pos[:, None] * inv_freq[None, :]).astype(np.float32).astype(np.float64)
    rope = np.zeros((128, 2, SEQ), np.float32)
    rope[:, 0, :] = 1.0
    for p in range(128):
        j = p % 64
        if j < 16:
            i = j % 8
            rope[p, 0, :] = np.cos(ang[:, i])
            rope[p, 1, :] = (-np.sin(ang[:, i])) if j < 8 else np.sin(ang[:, i])
    return cm.reshape(128, 4 * 128), rope.reshape(128, 2 * SEQ)


class Builder:
    def __init__(self, nseq=2, layers=(0, 1, 2, 3), nhalf=2, dbg=False, parts=("ffn1", "mixer", "ffn2")):
        self.dbg = dbg
        self.parts = parts
        self.stop_after = None
        self.wtot_override = None
        self.nseq = nseq
        self.layers = tuple(layers)
        self.nhalf = nhalf
        self.WL, self.WTOT = wlayout()
        self.PL, self.NP = playout()

    def sb(self, name, shape, dt):
        return self.st.enter_context(self.nc.sbuf_tensor(name, shape, dt))

    def ps(self, hold=False):
        while True:
            i = self.ps_set[self.ps_next % len(self.ps_set)]
            self.ps_next += 1
            if i not in self.ps_held:
                break
        if hold:
            self.ps_held.add(i)
        return self.PS[i], self.bPS[i]

    def ps_release(self, t):
        for i in range(8):
            if self.PS[i] is t:
                self.ps_held.discard(i)

    def rot(self, poolname):
        tiles, bufs, st = self.pools[poolname]
        i = st[0] % len(tiles)
        st[0] += 1
        return tiles[i], bufs[i]

    def mkpool(self, name, n, shape, dt):
        tiles = [self.sb("%s%d" % (name, i), shape, dt) for i in range(n)]
        bufs = [Buf("%s%d" % (name, i)) for i in range(n)]
        self.pools[name] = (tiles, bufs, [0])

    def wload(self, key):
        off, n = self.WL[key]
        i = self.w_next % N_WSLOT
        self.w_next += 1
        t, b = self.WS[i], self.bWS[i]
        self.P.dma("pool", M("dma_start", out=t[:, 0:n], in_=self.wall[:, off:off + n]),
                   writes=[b])
        return t, b

    def pcol(self, key, c=0):
        o = self.PL[key] + c
        return self.pvec[:, o:o + 1]

    def arena_reset(self):
        last = {}
        dmas = []
        for b in self.arena_bufs:
            ops = list(b.r)
            if b.w is not None:
                ops.append(b.w)
            for op in ops:
                if op.is_dma:
                    dmas.append(op)
                else:
                    cur = last.get(op.eng)
                    if cur is None or op.idx > cur.idx:
                        last[op.eng] = op
        self.arena_haz = list(last.values()) + dmas
        self.arena_bufs = []

    def abuf(self, name):
        b = Buf(name)
        b.r = list(self.arena_haz)
        self.arena_bufs.append(b)
        return b

    def build(self):
        nc = bass.Bass("TRN2", target_bir_lowering=False)
        self.nc = nc
        self.xs = nc.dram_tensor("xs", [self.nseq, SEQ, DM], F32, kind="ExternalInput").ap()
        self.wall = nc.dram_tensor("wall", [128, self.wtot_override or self.WTOT], F32, kind="ExternalInput").ap()
        self.pv_d = nc.dram_tensor("pv_in", [128, self.NP], F32, kind="ExternalInput").ap()
        self.wsm_d = nc.dram_tensor("wsm_in", [128, 2048], F32, kind="ExternalInput").ap()
        self.cm_d = nc.dram_tensor("cm_in", [128, 512], F32, kind="ExternalInput").ap()
        self.rope_d = nc.dram_tensor("rope_in", [128, 2 * SEQ], F32, kind="ExternalInput").ap()
        self.ys = nc.dram_tensor("ys", [self.nseq, SEQ, DM], F32, kind="ExternalOutput").ap()
        if self.dbg:
            self.dbg_d = nc.dram_tensor("dbg", [2, 128, 8 * SEG], F32, kind="ExternalOutput").ap()
        self.P = Prog(nc)
        self.pools = {}
        self.finals = []
        with ExitStack() as st:
            self.st = st
            self.alloc()
            self.setup()
            for s in range(self.nseq):
                self.seq_reset()
                for half in range(self.nhalf):
                    self.load_x(s, half)
                    sched = []
                    for l in self.layers:
                        if "ffn1" in self.parts:
                            sched.append(("ffn", l, 0))
                        if "mixer" in self.parts:
                            sched.append(("mix", l, 1))
                        if "ffn2" in self.parts:
                            sched.append(("ffn", l, 2))
                    if sched:
                        self.prenorm(sched[0][1], sched[0][2])
                    for i, (kind, l, w) in enumerate(sched):
                        self.next_norm = (sched[i + 1][1], sched[i + 1][2]) if i + 1 < len(sched) else None
                        if kind == "ffn":
                            self.ffn(l, w // 2, half)
                        elif l % 2 == 0:
                            self.mixer_even(l, half)
                        else:
                            self.mixer_odd(l, half)
                    self.store_x(s, half)
            self.P.emit(final_wait_ops=self.finals)
        return nc

    def alloc(self):
        nc = self.nc
        sb = self.sb
        self.X = sb("X", [128, 8, SEG], F32)
        self.bX = [[Buf("X%d_%d" % (c, t)) for t in range(2)] for c in range(8)]
        self.KTs = [sb("KTs%d" % e, [128, 4, SEG], BF16) for e in range(2)]
        self.bKTs = [[Buf("KTs%d_%d" % (e, c)) for c in range(4)] for e in range(2)]
        self.Vs = [sb("Vs%d" % e, [128, 8, 520], BF16) for e in range(2)]
        self.bVs = [[Buf("Vs%d_%d" % (e, k)) for k in range(8)] for e in range(2)]
        self.pvec = sb("pvec", [128, self.NP], F32)
        self.bpv = Buf("pvec")
        self.wsm = sb("wsm", [128, 16, 128], BF16)
        self.bwsm = Buf("wsm")
        self.cb = sb("cb", [128, 4, 128], BF16)
        self.bcb = Buf("cb")
        self.identf = sb("identf", [128, 128], F32)
        self.bident = Buf("identf")
        self.lc = sb("lc", [128, 2, 4, 2], F32)
        self.blc = Buf("lc")
        self.zh = sb("zh", [128, 2, 8, 2], F32)
        self.bzh = [[Buf("zh%d_%d" % (o, c)) for c in range(8)] for o in range(2)]
        self.lh = sb("lh", [128, 2, 4, 4], F32)
        self.blh = [[Buf("lh%d_%d" % (e, c)) for c in range(4)] for e in range(2)]
        self.hst = sb("hst", [128, 2, 4], F32)
        self.bhst = [[Buf("hst%d_%d" % (e, c)) for c in range(4)] for e in range(2)]
        self.kmx = sb("kmx", [128, 2, 4], F32)
        self.bkmx = [Buf("kmx%d" % e) for e in range(2)]
        self.small = sb("small", [128, 64], F32)
        self.H = sb("H", [128, 8, SEG], BF16)
        self.Ysb = self.H[:].rearrange("p c t -> p (c t)").bitcast(F32)
        self.bH = [[Buf("H%d_%d" % (c, t)) for t in range(2)] for c in range(8)]
        self.WS = [sb("ws%d" % i, [128, WSLOT], BF16) for i in range(N_WSLOT)]
        self.bWS = [Buf("ws%d" % i) for i in range(N_WSLOT)]
        self.w_next = 0
        self.mkpool("t32", 5, [128, TT], F32)
        self.mkpool("tb", 4, [128, TT], BF16)
        self.mkpool("rs", 2, [128, TT], F32)
        self.AR_BYTES = 77 * 1024
        self.AR = sb("arena", [128, self.AR_BYTES // 2], BF16)
        self.arena_bufs = []
        self.arena_haz = []
        self.PS = [self.st.enter_context(nc.psum_tensor("ps%d" % i, [128, TT], F32)) for i in range(8)]
        self.bPS = [Buf("ps%d" % i) for i in range(8)]
        self.ps_set = list(range(8))
        self.ps_next = 0
        self.ps_held = set()
        self.pending_stats = []

    def arena_view(self, byte_off, shape, dt):
        n = int(np.prod(shape[1:]))
        esz = 2 if dt == BF16 else 4
        assert byte_off % 4 == 0
        assert byte_off + n * esz <= self.AR_BYTES, (byte_off, n, esz)
        v = self.AR[:, byte_off // 2: byte_off // 2 + n * esz // 2]
        if dt == F32:
            v = v.bitcast(F32)
        return v

    def setup(self):
        P = self.P
        P.dma("sp", M("dma_start", out=self.pvec[:], in_=self.pv_d), writes=[self.bpv])
        P.dma("sp", M("dma_start", out=self.identf[:], in_=self.cm_d[:, 0:128]), writes=[self.bident])
        P.dma("pool", M("dma_start", out=self.wsm[:], in_=self.wsm_d.rearrange("p (a b) -> p a b", a=16)),
              writes=[self.bwsm])
        P.dma("pool", M("dma_start", out=self.cb[:], in_=self.cm_d.rearrange("p (a b) -> p a b", a=4)),
              writes=[self.bcb])
        bsm = Buf("small")
        for e in range(2):
            lam = self.pvec[:, self.PL[("lam", e)]:self.PL[("lam", e)] + 4]
            t1 = self.small[:, 0:4]
            t2 = self.small[:, 4:8]
            P.act(M("activation", out=t1, in_=lam, func=AF.Exp, scale=-1.0),
                  reads=[self.bpv], writes=[bsm])
            P.act(M("activation", out=t2, in_=t1, func=AF.Ln, bias=1.0),
                  reads=[bsm], writes=[bsm])
            P.dve(M("tensor_scalar", out=self.lc[:, e, :, 0], in0=t2, scalar1=-8.0, scalar2=None,
                                                         op0=ALU.mult), reads=[bsm], writes=[self.blc])
            P.dve(M("tensor_scalar", out=self.lc[:, e, :, 1], in0=t2, scalar1=-16.0, scalar2=None,
                                                         op0=ALU.mult), reads=[bsm], writes=[self.blc])
        for e in range(2):
            v4 = self.Vs[e][:].rearrange("p k (h d) -> p k h d", h=8)
            P.dve(M("memset", v4[:, :, :, 64:65], 1.0), writes=self.bVs[e])

    def seq_reset(self):
        P = self.P
        for o in range(2):
            P.dve(M("memset", self.zh[:, o], 0.0), writes=self.bzh[o])
        for e in range(2):
            P.dve(M("memset", self.lh[:, e], 0.0), writes=self.blh[e])
            P.dve(M("memset", self.hst[:, e], 0.0), writes=self.bhst[e])

    def stage_tiles(self):
        self.arena_reset()
        tiles = [self.arena_view(i * 4096, [128, DM], F32) for i in range(2)]
        bufs = [self.abuf("stage%d" % i) for i in range(2)]
        return tiles, bufs

    def load_x(self, s, half):
        P = self.P
        P.phase = "io"
        stiles, sbufs = self.stage_tiles()
        for tk in range(8):
            t0 = half * SEG + tk * 128
            stg, bst = stiles[tk % 2], sbufs[tk % 2]
            P.dma("sp", M("dma_start", out=stg, in_=self.xs[s, t0:t0 + 128, :]), writes=[bst])
            for cg in range(2):
                pt, bpt = self.ps()
                for ci in range(4):
                    c = cg * 4 + ci
                    P.pe(M("transpose", out=pt[:, ci * 128:(ci + 1) * 128],
                                                                         in_=stg[:, c * 128:(c + 1) * 128],
                                                                         identity=self.identf[:]),
                         reads=[bst, self.bident], writes=[bpt], accum=(ci > 0))
                tt = tk // 4
                col = tk * 128
                eng = P.act if cg == 0 else P.dve
                if cg == 0:
                    P.act(M("activation",
                        out=self.X[:, cg * 4:(cg + 1) * 4, col:col + 128],
                        in_=pt[:].rearrange("p (a b) -> p a b", a=4), func=AF.Copy),
                        reads=[bpt], writes=[self.bX[c][tt] for c in range(cg * 4, cg * 4 + 4)])
                else:
                    P.dve(M("tensor_copy",
                        out=self.X[:, cg * 4:(cg + 1) * 4, col:col + 128],
                        in_=pt[:].rearrange("p (a b) -> p a b", a=4)),
                        reads=[bpt], writes=[self.bX[c][tt] for c in range(cg * 4, cg * 4 + 4)])

    def store_x(self, s, half):
        P = self.P
        P.phase = "io"
        stiles, sbufs = self.stage_tiles()
        for tk in range(8):
            t0 = half * SEG + tk * 128
            tt = tk // 4
            col = tk * 128
            stg, bst = stiles[tk % 2], sbufs[tk % 2]
            for cg in range(2):
                pt, bpt = self.ps()
                for ci in range(4):
                    c = cg * 4 + ci
                    P.pe(M("transpose", out=pt[:, ci * 128:(ci + 1) * 128],
                                                                         in_=self.X[:, c, col:col + 128],
                                                                         identity=self.identf[:]),
                         reads=[self.bX[c][tt], self.bident], writes=[bpt], accum=(ci > 0))
                if cg == 0:
                    P.act(M("activation", out=stg[:, 0:512], in_=pt[:], func=AF.Copy),
                          reads=[bpt], writes=[bst])
                else:
                    P.dve(M("tensor_copy", out=stg[:, 512:1024], in_=pt[:]),
                          reads=[bpt], writes=[bst])
            d = P.dma("sp", M("dma_start", out=self.ys[s, t0:t0 + 128, :], in_=stg), reads=[bst])
            self.finals.append(d)

    def rstd_from(self, sps, bsps, hw):
        P = self.P
        sd, bsd = self.rot("t32")
        P.act(M("activation", out=sd[:], in_=sps[:], func=AF.Sqrt, scale=1.0 / (DM * hw * hw), bias=EPS / (hw * hw)),
              reads=[bsps], writes=[bsd])
        rs, brs = self.rot("rs")
        P.dve(M("reciprocal", out=rs[:], in_=sd[:]), reads=[bsd], writes=[brs])
        return rs, brs

    def prenorm_tt(self, l, w, tt):
        P = self.P
        ph = P.phase
        P.phase = "pre"
        tsl = slice(tt * TT, (tt + 1) * TT)
        sps, bsps = self.ps(hold=True)
        for c in range(8):
            sq, bsq = self.rot("tb")
            P.act(M("activation", out=sq[:], in_=self.X[:, c, tsl], func=AF.Square), reads=[self.bX[c][tt]], writes=[bsq])
            P.pe(M("matmul", sps[:], lhsT=self.cb[:, 1, :], rhs=sq[:], start=(c == 0), stop=(c == 7)),
                 reads=[bsq, self.bcb], writes=[bsps], accum=(c > 0))
        rs, brs = self.rstd_from(sps, bsps, 1.0)
        self.ps_release(sps)
        for c in range(8):
            g = self.pcol(("pre", l, w), c)
            t, bt = self.rot("t32")
            P.act(M("activation", out=t[:], in_=self.X[:, c, tsl], func=AF.Copy, scale=g), reads=[self.bX[c][tt], self.bpv], writes=[bt])
            P.dve(M("tensor_tensor", out=self.H[:, c, tsl], in0=t[:], in1=rs[:], op=ALU.mult), reads=[bt, brs], writes=[self.bH[c][tt]])
        P.phase = ph

    def prenorm(self, l, w):
        for tt in range(2):
            self.prenorm_tt(l, w, tt)

    def finish(self, l, w, spss, hw):
        for tt in range(2):
            self.postnorm(l, w, tt, spss[tt][0], spss[tt][1], hw)
            if self.next_norm is not None:
                self.prenorm_tt(self.next_norm[0], self.next_norm[1], tt)

    def ybuf(self, tt, c):
        if tt == 0:
            return self.Ysb[:, c * TT:(c + 1) * TT], [self.bH[c][0], self.bH[c][1]]
        return self.Ysb2[:, c * TT:(c + 1) * TT], [self.bY2[c]]

    def evac_y(self, py, bpy, c, tt, sps, bsps):
        P = self.P
        yv, yb = self.ybuf(tt, c)
        P.dve(M("tensor_copy", out=yv, in_=py[:]), reads=[bpy], writes=yb)
        sq, bsq = self.rot("tb")
        P.act(M("activation", out=sq[:], in_=yv, func=AF.Square), reads=yb, writes=[bsq])
        self.pending_stats.append((M("matmul", sps[:], lhsT=self.cb[:, 1, :], rhs=sq[:], start=(c == 0), stop=(c == 7)),
                                   [bsq, self.bcb], [bsps], c > 0))

    def flush_stats(self, keep=0):
        while len(self.pending_stats) > keep:
            fn, rd, wr, acc = self.pending_stats.pop(0)
            self.P.pe(fn, reads=rd, writes=wr, accum=acc)

    def postnorm(self, l, w, tt, sps, bsps, hw):
        P = self.P
        tsl = slice(tt * TT, (tt + 1) * TT)
        rs, brs = self.rstd_from(sps, bsps, hw)
        self.ps_release(sps)
        for c in range(8):
            g = self.pcol(("post", l, w), c)
            t, bt = self.rot("t32")
            yv, yb = self.ybuf(tt, c)
            P.act(M("activation", out=t[:], in_=yv, func=AF.Copy, scale=g), reads=yb + [self.bpv], writes=[bt])
            P.dve(M("tensor_tensor", out=t[:], in0=t[:], in1=rs[:], op=ALU.mult), reads=[bt, brs], writes=[bt])
            P.dve(M("tensor_tensor", out=self.X[:, c, tsl], in0=self.X[:, c, tsl], in1=t[:], op=ALU.add),
                  reads=[self.bX[c][tt], bt], writes=[self.bX[c][tt]])

    def make_ysb2(self, byte_off):
        self.Ysb2 = self.arena_view(byte_off, [128, 8 * TT], F32)
        haz = {}
        dmas = []
        for b in self.arena_bufs:
            ops = list(b.r)
            if b.w is not None:
                ops.append(b.w)
            for op in ops:
                if op.is_dma:
                    dmas.append(op)
                else:
                    cur = haz.get(op.eng)
                    if cur is None or op.idx > cur.idx:
                        haz[op.eng] = op
        hz = list(haz.values()) + dmas + list(self.arena_haz)
        self.bY2 = []
        for c in range(8):
            b = Buf("Y2_%d" % c)
            b.r = list(hz)
            self.arena_bufs.append(b)
            self.bY2.append(b)

    def ffn(self, l, f, half):
        P = self.P
        w = 0 if f == 0 else 2
        self.ps_set = list(range(8))
        P.phase = "ffnP1"
        self.arena_reset()
        A = self.arena_view(0, [128, NJ, SEG], BF16).rearrange("p (j t) -> p j t", j=NJ)
        bA = [[self.abuf("A%d_%d" % (j, t)) for t in range(2)] for j in range(NJ)]
        for jp in range(NJ // 2):
            wvs = []
            for j in (2 * jp, 2 * jp + 1):
                wt, bw = self.wload(("gu", l, f, j))
                wvs.append((j, wt[:, 0:2048].rearrange("p (s c f) -> p s c f", s=2, c=8), bw))
            for tt in range(2):
                tsl = slice(tt * TT, (tt + 1) * TT)
                for j, wv, bw in wvs:
                    pg, bg = self.ps()
                    pu, bu = self.ps()
                    for s_, (pp, bp) in enumerate(((pg, bg), (pu, bu))):
                        for c in range(8):
                            P.pe(M("matmul", pp[:], lhsT=wv[:, s_, c, :], rhs=self.H[:, c, tsl], start=(c == 0), stop=(c == 7)),
                                 reads=[bw, self.bH[c][tt]], writes=[bp], accum=(c > 0))
                    sg, bsg = self.rot("t32")
                    P.act(M("activation", out=sg[:], in_=pg[:], func=AF.Silu), reads=[bg], writes=[bsg])
                    P.dve(M("tensor_tensor", out=A[:, j, tsl], in0=sg[:], in1=pu[:], op=ALU.mult),
                          reads=[bsg, bu], writes=[bA[j][tt]])
        if self.stop_after == "phase1":
            return
        P.phase = "ffnP2"
        self.make_ysb2(NJ * SEG * 2)
        spss = [self.ps(hold=True) for _ in range(2)]
        order = [(c, tt) for c in range(6) for tt in range(2)] + [(6, 0), (7, 0), (6, 1), (7, 1)]
        wmap = {}
        for c, tt in order:
            if c not in wmap:
                wt, bw = self.wload(("d", l, f, c))
                wmap[c] = (wt[:, 0:2816].rearrange("p (j f) -> p j f", j=NJ), bw)
            wv, bw = wmap[c]
            tsl = slice(tt * TT, (tt + 1) * TT)
            py, bpy = self.ps()
            for j in range(NJ):
                P.pe(M("matmul", py[:], lhsT=wv[:, j, :], rhs=A[:, j, tsl], start=(j == 0), stop=(j == NJ - 1)),
                     reads=[bw, bA[j][tt]], writes=[bpy], accum=(j > 0))
            self.flush_stats()
            self.evac_y(py, bpy, c, tt, spss[tt][0], spss[tt][1])
        self.flush_stats()
        self.finish(l, w, spss, 0.5)

    def out_proj(self, l, CAT, bCAT):
        P = self.P
        P.phase = "oproj"
        self.make_ysb2(16384)
        spss = [self.ps(hold=True) for _ in range(2)]
        order = [(cc, tt) for cc in range(6) for tt in range(2)] + [(6, 0), (7, 0), (6, 1), (7, 1)]
        wmap = {}
        for cc, tt in order:
            g, s_ = cc // 2, cc % 2
            if g not in wmap:
                wt, bw = self.wload(("out", l, g))
                wmap[g] = (wt[:, 0:2048].rearrange("p (s c f) -> p s c f", s=2, c=8), bw)
            wv, bw = wmap[g]
            tsl = slice(tt * TT, (tt + 1) * TT)
            py, bpy = self.ps()
            for c in range(8):
                P.pe(M("matmul", py[:], lhsT=wv[:, s_, c, :], rhs=CAT[:, c, tsl], start=(c == 0), stop=(c == 7)),
                     reads=[bw, bCAT[c][tt]], writes=[bpy], accum=(c > 0))
            self.flush_stats()
            self.evac_y(py, bpy, cc, tt, spss[tt][0], spss[tt][1])
        self.flush_stats()
        self.finish(l, 1, spss, 1.0)

    def mixer_odd(self, l, half):
        P = self.P
        o = l // 2
        self.ps_set = list(range(8))
        P.phase = "odd"
        self.arena_reset()
        CAT = self.arena_view(0, [128, 8, SEG], BF16).rearrange("p (c t) -> p c t", c=8)
        bCAT = [[self.abuf("CAT%d_%d" % (c, t)) for t in range(2)] for c in range(8)]
        Z = [self.arena_view(16384 + i * 2064, [128, 516], F32) for i in range(2)]
        bZ = [self.abuf("Z%d" % i) for i in range(2)]
        zi = 0
        for c in range(8):
            wt, bw = self.wload(("cin", o, c))
            wv = wt[:, 0:3072].rearrange("p (s c f) -> p s c f", s=3, c=8)
            for tt in range(2):
                tsl = slice(tt * TT, (tt + 1) * TT)
                pps = [self.ps() for _ in range(3)]
                for s_ in range(3):
                    pp, bp = pps[s_]
                    for k in range(8):
                        P.pe(M("matmul",
                            pp[:], lhsT=wv[:, s_, k, :], rhs=self.H[:, k, tsl], start=(k == 0), stop=(k == 7)),
                            reads=[bw, self.bH[k][tt]], writes=[bp], accum=(k > 0))
                (pc, bpc), (px, bpx), (pb, bpb) = pps
                z, bz = Z[zi % 2], bZ[zi % 2]
                zi += 1
                zc, bzc = self.rot("t32")
                P.act(M("activation", out=zc[:], in_=pc[:], func=AF.Copy), reads=[bpc], writes=[bzc])
                P.dve(M("tensor_copy", out=z[:, 0:2], in_=self.zh[:, o, c, :]), reads=[self.bzh[o][c]], writes=[bz])
                P.dve(M("tensor_tensor", out=z[:, 2:514], in0=zc[:], in1=px[:], op=ALU.mult),
                      reads=[bzc, bpx, bz], writes=[bz])
                P.dve(M("tensor_copy", out=self.zh[:, o, c, :], in_=z[:, 512:514]), reads=[bz], writes=[self.bzh[o][c]])
                y, by = self.rot("t32")
                P.dve(M("tensor_scalar", out=y[:], in0=z[:, 0:512], scalar1=self.pcol(("ccw", o, 0), c),
                                                           scalar2=None, op0=ALU.mult), reads=[bz, self.bpv], writes=[by])
                for j in (1, 2):
                    P.dve(M("scalar_tensor_tensor",
                        out=y[:], in0=z[:, j:j + 512], scalar=self.pcol(("ccw", o, j), c), in1=y[:], op0=ALU.mult, op1=ALU.add),
                        reads=[bz, by, self.bpv], writes=[by])
                P.dve(M("tensor_tensor", out=CAT[:, c, tsl], in0=y[:], in1=pb[:], op=ALU.mult),
                      reads=[by, bpb], writes=[bCAT[c][tt]])
        self.out_proj(l, CAT, bCAT)

    def mixer_even(self, l, half):
        P = self.P
        e_ = l // 2
        self.ps_set = list(range(8))
        P.phase = "lru"
        self.arena_reset()
        off = [0]

        def carve(shape, dt):
            n = int(np.prod(shape[1:])) * (2 if dt == BF16 else 4)
            v = self.arena_view(off[0], shape, dt)
            off[0] += (n + 3) // 4 * 4
            return v
        CAT = carve([128, 8, SEG], BF16).rearrange("p (c t) -> p c t", c=8)
        bCAT = [[self.abuf("CAT%d_%d" % (c, t)) for t in range(2)] for c in range(8)]
        QT = carve([128, 4, SEG], BF16).rearrange("p (c t) -> p c t", c=4)
        bQT = [[self.abuf("QT%d_%d" % (c, t)) for t in range(2)] for c in range(4)]
        ropet = carve([128, 2, SEG], F32).rearrange("p (a t) -> p a t", a=2)
        brope = self.abuf("rope")
        if half == 0:
            KTc, bKTc = self.KTs[e_], self.bKTs[e_]
            Vc, bVc = self.Vs[e_], self.bVs[e_]
        else:
            KTc = carve([128, 4, SEG], BF16).rearrange("p (c t) -> p c t", c=4)
            bKTc = [self.abuf("KTc%d" % c) for c in range(4)]
            Vc = carve([128, 8, 520], BF16).rearrange("p (k d) -> p k d", k=8)
            bVc = [self.abuf("Vc%d" % k) for k in range(8)]
            v4 = Vc.rearrange("p k (h d) -> p k h d", h=8)
            P.dve(M("memset", v4[:, :, :, 64:65], 1.0), writes=bVc)
        XR = carve([128, 516], F32)
        bXR = self.abuf("XR")
        lt = [carve([128, TT], F32) for _ in range(6)]
        blt = [self.abuf("lt%d" % i) for i in range(6)]
        xcb = carve([128, TT], BF16)
        bxcb = self.abuf("xcb")
        NPT = 6
        PT = [carve([128, 256], BF16) for _ in range(NPT)]
        bPT = [self.abuf("PT%d" % i) for i in range(NPT)]
        osb = [carve([128, 8, 65], F32).rearrange("p (n d) -> p n d", n=8) for _ in range(2)]
        bosb = [self.abuf("osb%d" % i) for i in range(2)]
        atok = [carve([128, 512], F32) for _ in range(2)]
        batok = [self.abuf("atok%d" % i) for i in range(2)]
        acc = [carve([128, 68], F32) for _ in range(2)]
        bacc = [self.abuf("acc%d" % i) for i in range(2)]
        KMb = carve([128, 4, 8], BF16).rearrange("p (c n) -> p c n", c=4)
        bKM = self.abuf("KM")
        KMf = carve([128, 4, 8], F32).rearrange("p (c n) -> p c n", c=4)
        bKMf = self.abuf("KMf")
        g8 = [carve([128, 8], F32) for _ in range(4)]
        bg8 = [self.abuf("g8_%d" % i) for i in range(4)]
        top8 = [carve([128, 8], F32) for _ in range(4)]
        btop8 = [self.abuf("top8_%d" % i) for i in range(4)]
        sel = [carve([128, 8], F32) for _ in range(4)]
        bsel = [self.abuf("sel%d" % i) for i in range(4)]
        qm = carve([128, 4, 2], F32).rearrange("p (c t) -> p c t", c=4)
        bqm = self.abuf("qm")
        km = carve([128, 4, 2], F32).rearrange("p (c t) -> p c t", c=4)
        bkm = self.abuf("km")
        negM = carve([128, 4], F32)
        bnegM = self.abuf("negM")
        sm1 = carve([128, 4], F32)
        sm2 = carve([128, 4], F32)
        bsm = self.abuf("sm")
        rcp = [carve([128, 2], F32) for _ in range(2)]
        brcp = [self.abuf("rcp%d" % i) for i in range(2)]

        pos0 = half * SEG
        P.dma("sp", M("dma_start", out=ropet, in_=self.rope_d.rearrange("p (a t) -> p a t", a=2)[:, :, pos0:pos0 + SEG]),
              writes=[brope])

        gg, xc, r_, i_, a_, s2 = lt
        bgg, bxc, br, bi, ba, bs2 = blt
        for c in range(4):
            wt, bw = self.wload(("lru", e_, c))
            wv = wt[:, 0:2048].rearrange("p (s c f) -> p s c f", s=2, c=8)
            wtq, bwq = self.wload(("qk", e_, c))
            wvq = wtq[:, 0:2048].rearrange("p (s c f) -> p s c f", s=2, c=8)
            for tt in range(2):
                tsl = slice(tt * TT, (tt + 1) * TT)
                P.phase = "lru"
                pgg, bpg = self.ps()
                pxx, bpx = self.ps()
                for s_, (pp, bp) in enumerate(((pgg, bpg), (pxx, bpx))):
                    for k in range(8):
                        P.pe(M("matmul", pp[:], lhsT=wv[:, s_, k, :], rhs=self.H[:, k, tsl], start=(k == 0), stop=(k == 7)),
                             reads=[bw, self.bH[k][tt]], writes=[bp], accum=(k > 0))
                P.act(M("activation", out=gg[:], in_=pgg[:], func=AF.Gelu_apprx_tanh), reads=[bpg], writes=[bgg])
                P.dve(M("tensor_copy", out=XR[:, 0:3], in_=self.lh[:, e_, c, 0:3]), reads=[self.blh[e_][c]], writes=[bXR])
                P.act(M("activation", out=XR[:, 3:515], in_=pxx[:], func=AF.Copy), reads=[bpx, bXR], writes=[bXR])
                P.dve(M("tensor_copy", out=self.lh[:, e_, c, 0:3], in_=XR[:, 512:515]), reads=[bXR], writes=[self.blh[e_][c]])
                P.phase = "qk"
                pqk = []
                for s_ in range(2):
                    pq, bpq = self.ps()
                    for k in range(8):
                        P.pe(M("matmul", pq[:], lhsT=wvq[:, s_, k, :], rhs=self.H[:, k, tsl], start=(k == 0), stop=(k == 7)),
                             reads=[bwq, self.bH[k][tt]], writes=[bpq], accum=(k > 0))
                    qraw, bqraw = self.rot("tb")
                    P.act(M("activation", out=qraw[:], in_=pq[:], func=AF.Copy), reads=[bpq], writes=[bqraw])
                    pqk.append((pq, bpq, qraw, bqraw))
                P.phase = "lru"
                P.dve(M("tensor_scalar", out=xc[:], in0=XR[:, 0:512], scalar1=self.pcol(("lcw", e_, 0), c),
                        scalar2=self.pcol(("lcb", e_), c), op0=ALU.mult, op1=ALU.add), reads=[bXR, self.bpv], writes=[bxc])
                for j in (1, 2, 3):
                    P.dve(M("scalar_tensor_tensor", out=xc[:], in0=XR[:, j:j + 512], scalar=self.pcol(("lcw", e_, j), c), in1=xc[:],
                            op0=ALU.mult, op1=ALU.add), reads=[bXR, bxc, self.bpv], writes=[bxc])
                P.phase = "qk"
                prots = []
                for s_ in range(2):
                    pq, bpq, qraw, bqraw = pqk[s_]
                    prot, bprot = self.ps()
                    P.pe(M("matmul", prot[:], lhsT=self.cb[:, 2, :], rhs=qraw[:], start=True, stop=True),
                         reads=[bqraw, self.bcb], writes=[bprot])
                    prots.append((prot, bprot))
                P.phase = "lru"
                P.act(M("activation", out=xcb[:], in_=xc[:], func=AF.Copy), reads=[bxc], writes=[bxcb])
                pr, bpr = self.ps()
                pi, bpi = self.ps()
                P.pe(M("matmul", pr[:], lhsT=self.wsm[:, (e_ * 2 + 0) * 4 + c, :], rhs=xcb[:], start=True, stop=True),
                     reads=[bxcb, self.bwsm], writes=[bpr])
                P.pe(M("matmul", pi[:], lhsT=self.wsm[:, (e_ * 2 + 1) * 4 + c, :], rhs=xcb[:], start=True, stop=True),
                     reads=[bxcb, self.bwsm], writes=[bpi])
                P.phase = "qk"
                for s_ in range(2):
                    pq, bpq, qraw, bqraw = pqk[s_]
                    prot, bprot = prots[s_]
                    t1, bt1 = self.rot("t32")
                    t2, bt2 = self.rot("t32")
                    P.dve(M("tensor_tensor", out=t1[:], in0=pq[:], in1=ropet[:, 0, tsl], op=ALU.mult),
                          reads=[bpq, brope, bqraw], writes=[bt1])
                    P.dve(M("tensor_tensor", out=t2[:], in0=prot[:], in1=ropet[:, 1, tsl], op=ALU.mult),
                          reads=[bprot, brope], writes=[bt2])
                    if s_ == 0:
                        dst, bd = QT[:, c, tsl], bQT[c][tt]
                    else:
                        dst, bd = KTc[:, c, tsl], bKTc[c]
                    P.dve(M("tensor_tensor", out=dst, in0=t1[:], in1=t2[:], op=ALU.add), reads=[bt1, bt2], writes=[bd])
                P.phase = "lru"
                P.act(M("activation", out=r_[:], in_=pr[:], func=AF.Sigmoid, bias=self.pcol(("ba", e_), c)),
                      reads=[bpr, self.bpv], writes=[br])
                P.act(M("activation", out=i_[:], in_=pi[:], func=AF.Sigmoid, bias=self.pcol(("bx", e_), c)),
                      reads=[bpi, self.bpv], writes=[bi])
                P.act(M("activation", out=a_[:], in_=r_[:], func=AF.Exp, scale=self.lc[:, e_, c, 0:1]), reads=[br, self.blc], writes=[ba])
                P.act(M("activation", out=s2[:], in_=r_[:], func=AF.Exp, scale=self.lc[:, e_, c, 1:2]), reads=[br, self.blc], writes=[bs2])
                P.act(M("activation", out=s2[:], in_=s2[:], func=AF.Sqrt, scale=-1.0, bias=1.0), reads=[bs2], writes=[bs2])
                P.dve(M("tensor_tensor", out=i_[:], in0=i_[:], in1=xc[:], op=ALU.mult), reads=[bi, bxc], writes=[bi])
                P.dve(M("tensor_tensor", out=i_[:], in0=i_[:], in1=s2[:], op=ALU.mult), reads=[bi, bs2], writes=[bi])
                P.dve(M("tensor_tensor_scan", out=r_[:], data0=a_[:], data1=i_[:], initial=self.hst[:, e_, c:c + 1],
                        op0=ALU.mult, op1=ALU.add), reads=[ba, bi, self.bhst[e_][c], br], writes=[br])
                P.dve(M("tensor_copy", out=self.hst[:, e_, c:c + 1], in_=r_[:, TT - 1:TT]), reads=[br], writes=[self.bhst[e_][c]])
                P.dve(M("tensor_tensor", out=CAT[:, 4 + c, tsl], in0=r_[:], in1=gg[:], op=ALU.mult),
                      reads=[br, bgg], writes=[bCAT[4 + c][tt]])

        P.phase = "v"
        for vh in range(2):
            wt, bw = self.wload(("v", e_, vh))
            wv = wt[:, 0:2048].rearrange("p (c f) -> p c f", c=8)
            for tk in range(8):
                pv_, bpv_ = self.ps()
                for k in range(8):
                    P.pe(M("matmul",
                        pv_[:, 0:256], lhsT=self.H[:, k, tk * 128:(tk + 1) * 128], rhs=wv[:, k, :], start=(k == 0), stop=(k == 7)),
                        reads=[bw, self.bH[k][tk // 4]], writes=[bpv_], accum=(k > 0))
                dst = Vc[:, tk, :].rearrange("p (h d) -> p h d", h=8)[:, vh * 4:(vh + 1) * 4, 0:64]
                P.act(M("activation", out=dst, in_=pv_[:, 0:256].rearrange("p (h d) -> p h d", h=4), func=AF.Copy),
                      reads=[bpv_], writes=[bVc[tk]])

        P.phase = "attn"
        scale = 0.125
        for c in range(4):
            for which, (src, bsrc, dstm, bdm) in enumerate(((QT, None, qm, bqm), (KTc, None, km, bkm))):
                for tt in range(2):
                    tsl = slice(tt * TT, (tt + 1) * TT)
                    rb = [bQT[c][tt]] if which == 0 else [bKTc[c]]
                    sq, bsq = self.rot("tb")
                    P.act(M("activation", out=sq[:], in_=src[:, c, tsl], func=AF.Square),
                          reads=rb, writes=[bsq])
                    pn, bpn = self.ps()
                    P.pe(M("matmul", pn[:], lhsT=self.cb[:, 1, :], rhs=sq[:], start=True, stop=True),
                         reads=[bsq, self.bcb], writes=[bpn])
                    P.dve(M("tensor_reduce", out=dstm[:, c, tt:tt + 1], in_=pn[:], axis=AX.X, op=ALU.max),
                          reads=[bpn], writes=[bdm])
        P.dve(M("tensor_tensor", out=sm1[:], in0=qm[:, :, 0], in1=qm[:, :, 1], op=ALU.max), reads=[bqm], writes=[bsm])
        P.dve(M("tensor_tensor", out=sm2[:], in0=km[:, :, 0], in1=km[:, :, 1], op=ALU.max), reads=[bkm, bsm], writes=[bsm])
        if half == 0:
            P.dve(M("tensor_copy", out=self.kmx[:, e_, :], in_=sm2[:]), reads=[bsm], writes=[self.bkmx[e_]])
        else:
            P.dve(M("tensor_tensor", out=sm2[:], in0=sm2[:], in1=self.kmx[:, e_, :], op=ALU.max),
                  reads=[bsm, self.bkmx[e_]], writes=[bsm])
        P.dve(M("tensor_tensor", out=sm1[:], in0=sm1[:], in1=sm2[:], op=ALU.mult), reads=[bsm], writes=[bsm])
        P.act(M("activation", out=sm1[:], in_=sm1[:], func=AF.Sqrt), reads=[bsm], writes=[bsm])
        P.dve(M("tensor_scalar", out=negM[:], in0=sm1[:], scalar1=-scale, scalar2=None, op0=ALU.mult), reads=[bsm], writes=[bnegM])

        def kt_src(kc):
            if kc < 8 and half == 1:
                return self.KTs[e_], self.bKTs[e_], kc
            return KTc, bKTc, kc % 8

        def v_src(kc):
            if kc < 8 and half == 1:
                return self.Vs[e_], self.bVs[e_], kc
            return Vc, bVc, kc % 8

        if half == 1:
            for c in range(4):
                P.dve(M("tensor_reduce", out=KMf[:, c, 0:4], in_=self.KTs[e_][:, c, :].rearrange("p (n k) -> p n k", n=4),
                                                     axis=AX.X, op=ALU.add), reads=[self.bKTs[e_][c]], writes=[bKMf])
                P.dve(M("tensor_reduce", out=KMf[:, c, 4:8], in_=KTc[:, c, :].rearrange("p (n k) -> p n k", n=4),
                                                     axis=AX.X, op=ALU.add), reads=[bKTc[c]], writes=[bKMf])
            P.dve(M("tensor_scalar", out=KMb[:], in0=KMf[:], scalar1=1.0 / 256, scalar2=None, op0=ALU.mult),
                  reads=[bKMf], writes=[bKM])

        self.ps_set = [4, 5, 6, 7]
        OB = [self.PS[i] for i in range(4)]
        bOB = [self.bPS[i] for i in range(4)]
        pti = [0]
        LAG = 2
        for qg in range(4):
            own = 4 * half + qg
            q0 = qg * 256
            nkc = 2 * own + 2
            use_sel = own >= 4

            def emit_sel(hd):
                c = hd // 2
                hp = 64 * (hd % 2)
                par = hd % 2
                for qt in range(2):
                    k_ = qt * 2 + par
                    pgt, bpgt = self.ps()
                    P.pe(M("matmul", pgt[:, 0:8], lhsT=QT[hp:hp + 64, c, q0 + qt * 128:q0 + (qt + 1) * 128],
                           rhs=KMb[hp:hp + 64, c, :], start=True, stop=True), reads=[bQT[c][qg // 2], bKM], writes=[bpgt])
                    P.dve(M("memset", g8[k_][:], -1e30), writes=[bg8[k_]])
                    P.dve(M("tensor_copy", out=g8[k_][:, 0:own], in_=pgt[:, 0:own]), reads=[bpgt, bg8[k_]], writes=[bg8[k_]])
                    P.dve(M("max", out=top8[k_][:], in_=g8[k_][:]), reads=[bg8[k_]], writes=[btop8[k_]])
                    P.dve(M("tensor_scalar", out=sel[k_][:], in0=g8[k_][:], scalar1=top8[k_][:, 2:3], scalar2=None, op0=ALU.is_ge),
                          reads=[bg8[k_], btop8[k_]], writes=[bsel[k_]])
                    P.dve(M("memset", sel[k_][:, own:own + 1], 1.0), reads=[bsel[k_]], writes=[bsel[k_]])

            def emit_S(hd, kc):
                c = hd // 2
                hp = 64 * (hd % 2)
                ktt, bkt, kl = kt_src(kc)
                dq = kc - 2 * own
                qlo = 128 if dq == 1 else 0
                pS, bpS = self.ps(hold=True)
                P.pe(M("matmul", pS[:, qlo:256], lhsT=ktt[hp:hp + 64, c, kl * 128:(kl + 1) * 128],
                       rhs=QT[hp:hp + 64, c, q0 + qlo:q0 + 256], start=True, stop=True),
                     reads=[bkt[c], bQT[c][qg // 2]], writes=[bpS])
                return pS, bpS, qlo, dq

            def emit_EV(hd, kc, st_):
                pS, bpS, qlo, dq = st_
                c = hd // 2
                vt, bv, vl = v_src(kc)
                n = kc // 2
                pt_, bpt_ = PT[pti[0] % NPT], bPT[pti[0] % NPT]
                pti[0] += 1
                P.act(M("activation", out=pt_[:, qlo:256], in_=pS[:, qlo:256], func=AF.Exp, scale=scale, bias=negM[:, c:c + 1]),
                      reads=[bpS, bnegM], writes=[bpt_])
                self.ps_release(pS)
                if dq >= 0:
                    P.dve(M("tensor_tensor", out=pt_[:, qlo:qlo + 128], in0=pt_[:, qlo:qlo + 128], in1=self.cb[:, 3, :], op=ALU.mult),
                          reads=[bpt_, self.bcb], writes=[bpt_])
                for qt in range(2):
                    if qt * 128 < qlo:
                        continue
                    gq = 2 * own + qt
                    if use_sel:
                        reg = n
                        first = (kc % 2 == 0)
                        last = (kc % 2 == 1) or (kc == gq)
                        bank = qt * 2 + reg // 4
                    else:
                        reg = 0
                        first = (kc == 0)
                        last = (kc == gq)
                        bank = qt * 2 + hd % 2
                    ob, bob = OB[bank], bOB[bank]
                    P.pe(M("matmul", ob[:, (reg % 4) * 65:(reg % 4) * 65 + 65], lhsT=pt_[:, qt * 128:(qt + 1) * 128],
                           rhs=vt[:, vl, hd * 65:(hd + 1) * 65], start=first, stop=last),
                         reads=[bpt_, bv[vl]], writes=[bob], accum=(not first))

            def emit_combine(hd):
                par = hd % 2
                for qt in range(2):
                    ac, bac = acc[qt], bacc[qt]
                    if use_sel:
                        k_ = qt * 2 + par
                        nb = own + 1
                        P.dve(M("tensor_copy", out=osb[qt][:, 0:4, :], in_=OB[qt * 2][:, 0:260].rearrange("p (n d) -> p n d", n=4)),
                              reads=[bOB[qt * 2]], writes=[bosb[qt]])
                        P.dve(M("tensor_copy", out=osb[qt][:, 4:nb, :],
                                in_=OB[qt * 2 + 1][:, 0:(nb - 4) * 65].rearrange("p (n d) -> p n d", n=nb - 4)),
                              reads=[bOB[qt * 2 + 1], bosb[qt]], writes=[bosb[qt]])
                for qt in range(2):
                    ac, bac = acc[qt], bacc[qt]
                    if use_sel:
                        k_ = qt * 2 + par
                        nb = own + 1
                        P.dve(M("tensor_tensor", out=osb[qt][:, 0:nb, :], in0=osb[qt][:, 0:nb, :],
                                in1=sel[k_][:, 0:nb].unsqueeze(2).broadcast_to([128, nb, 65]), op=ALU.mult),
                              reads=[bosb[qt], bsel[k_]], writes=[bosb[qt]])
                        P.dve(M("tensor_reduce", out=ac[:, 0:65], in_=osb[qt][:, 0:nb, :].rearrange("p n d -> p d n"), axis=AX.X, op=ALU.add),
                              reads=[bosb[qt]], writes=[bac])
                    else:
                        ob, bob = OB[qt * 2 + par], bOB[qt * 2 + par]
                        P.dve(M("tensor_copy", out=ac[:, 0:65], in_=ob[:, 0:65]), reads=[bob], writes=[bac])
                    P.dve(M("reciprocal", out=rcp[qt][:, 0:1], in_=ac[:, 64:65]), reads=[bac], writes=[brcp[qt]])
                    P.dve(M("tensor_scalar", out=atok[qt][:, hd * 64:(hd + 1) * 64], in0=ac[:, 0:64], scalar1=rcp[qt][:, 0:1],
                            scalar2=None, op0=ALU.mult), reads=[bac, brcp[qt]], writes=[batok[qt]])

            seq_ = [(hd, kc) for hd in range(8) for kc in range(nkc)]
            states = {}
            for i in range(len(seq_) + LAG):
                if i < len(seq_):
                    hd, kc = seq_[i]
                    if kc == 0 and use_sel:
                        emit_sel(hd)
                    states[i] = emit_S(hd, kc)
                j = i - LAG
                if j >= 0:
                    hd, kc = seq_[j]
                    emit_EV(hd, kc, states.pop(j))
                    if kc == nkc - 1:
                        emit_combine(hd)
            for qt in range(2):
                ptp, bptp = self.ps()
                for cc in range(4):
                    P.pe(M("transpose", out=ptp[:, cc * 128:(cc + 1) * 128],
                                                                      in_=atok[qt][:, cc * 128:(cc + 1) * 128], identity=self.identf[:]),
                         reads=[batok[qt], self.bident], writes=[bptp], accum=(cc > 0))
                col = q0 + qt * 128
                P.act(M("activation", out=CAT[:, 0:4, col:col + 128],
                                                               in_=ptp[:].rearrange("p (a b) -> p a b", a=4), func=AF.Copy),
                      reads=[bptp], writes=[bCAT[cc][qg // 2] for cc in range(4)])
        self.ps_set = list(range(8))
        if self.dbg:
            d = P.dma("pool", M("dma_start", out=self.dbg_d[half].rearrange("p (c t) -> p c t", c=8), in_=CAT),
                      reads=[b for bb in bCAT for b in bb])
            self.finals.append(d)
        self.out_proj(l, CAT, bCAT)


_CACHE = {}


def _get_prog(nseq, layers, nhalf):
    key = (nseq, tuple(layers), nhalf)
    if key not in _CACHE:
        _CACHE[key] = Builder(nseq, layers, nhalf).build()
    return _CACHE[key]


def kernel(**inputs):
    inp = {k: np.asarray(v) for k, v in inputs.items()}
    x = np.ascontiguousarray(inp["x"], dtype=np.float32)
    wall = pack_weights(inp)
    pv, wsm = pack_small(inp)
    cm, rope = const_tables()
    nc = _get_prog(2, (0, 1, 2, 3), 2)
    in_maps = []
    for i in range(NCORES):
        in_maps.append({"xs": x[2 * i:2 * i + 2], "wall": wall, "pv_in": pv, "wsm_in": wsm, "cm_in": cm, "rope_in": rope})
    res = run_bass_kernel_spmd(nc, in_maps, core_ids=list(range(NCORES)))
    out = np.concatenate([r["ys"] for r in res.results], axis=0)
    return out.astype(np.float32)
```

```python
import numpy as np
from contextlib import ExitStack
import concourse.bass as bass
import concourse.mybir as mybir
from concourse.bass_utils import run_bass_kernel_spmd

F32 = mybir.dt.float32
BF16 = mybir.dt.bfloat16
AF = mybir.ActivationFunctionType
ALU = mybir.AluOpType
AX = mybir.AxisListType

ENGS = ("pe", "act", "dve", "pool", "sp")
NCORES = 8
SEQ = 2048
DM = 1024
DFF = 2816
NJ = 22
SEG = 1024
TT = 512
EPS = 1e-6
ROPE_THETA = 500000.0
WSLOT = 3072
N_WSLOT = 4


def M(name, *a, **kw):
    return (name, a, kw)


class Buf:
    __slots__ = ("name", "w", "r")

    def __init__(self, name):
        self.name = name
        self.w = None
        self.r = []


class Op:
    __slots__ = ("eng", "fn", "deps", "mark", "semval", "is_dma", "dma_sem", "dma_val", "idx", "phase")


class Prog:
    def __init__(self, nc, per_q=10):
        self.nc = nc
        self.ops = {e: [] for e in ENGS}
        self.per_q = per_q
        self.dma_q = {"sp": 0, "act": 1, "pool": 2}
        self.n_dma_sems = per_q * 3
        self.dma_sem_next = {"sp": 0, "act": 0, "pool": 0}
        self.dma_counts = [0] * self.n_dma_sems
        self.all_ops = []
        self.annotate = getattr(Prog, 'annotate_default', False)
        self.phase = ""

    def _add(self, eng, fn, reads, writes, is_dma=False, accum=False):
        op = Op()
        op.eng = eng
        op.fn = fn
        op.mark = False
        op.semval = None
        op.is_dma = is_dma
        op.dma_sem = None
        op.dma_val = None
        op.idx = len(self.ops[eng])
        op.phase = getattr(self, "phase", "")
        deps = {}
        for b in reads:
            if b.w is not None:
                deps[id(b.w)] = b.w
        for b in writes:
            if b.w is not None:
                if not (accum and b.w.eng == "pe" and eng == "pe" and not b.w.is_dma):
                    deps[id(b.w)] = b.w
            for r in b.r:
                deps[id(r)] = r
        deps.pop(id(op), None)
        op.deps = [d for d in deps.values()
                   if not (d.eng == "pe" and eng == "pe" and not d.is_dma and not is_dma)]
        for b in reads:
            b.r.append(op)
        for b in writes:
            b.w = op
            b.r = []
        if is_dma:
            k = self.dma_sem_next[eng]
            self.dma_sem_next[eng] = (k + 1) % self.per_q
            s = self.dma_q[eng] * self.per_q + k
            self.dma_counts[s] += 16
            op.dma_sem = s
            op.dma_val = self.dma_counts[s]
        self.ops[eng].append(op)
        self.all_ops.append(op)
        return op

    def pe(self, fn, reads=(), writes=(), accum=False):
        return self._add("pe", fn, reads, writes, accum=accum)

    def act(self, fn, reads=(), writes=()):
        return self._add("act", fn, reads, writes)

    def dve(self, fn, reads=(), writes=()):
        return self._add("dve", fn, reads, writes)

    def pool(self, fn, reads=(), writes=()):
        return self._add("pool", fn, reads, writes)

    def dma(self, eng, fn, reads=(), writes=()):
        return self._add(eng, fn, reads, writes, is_dma=True)

    def emit(self, final_wait_ops=()):
        nc = self.nc
        for op in self.all_ops:
            for d in op.deps:
                if not d.is_dma:
                    d.mark = True
        for op in final_wait_ops:
            if not op.is_dma:
                op.mark = True
        cnt = {e: 0 for e in ENGS}
        for e in ENGS:
            for op in self.ops[e]:
                if op.mark and not op.is_dma:
                    cnt[e] += 1
                    op.semval = cnt[e]
        self.stats = {}
        with ExitStack() as st:
            esem = {e: st.enter_context(nc.semaphore("es_" + e)) for e in ENGS}
            dsem = [st.enter_context(nc.semaphore("ds%d" % i)) for i in range(self.n_dma_sems)]
            block = st.enter_context(nc.Block())

            def run(engname, engobj):
                known_e = {e: 0 for e in ENGS}
                known_d = [0] * self.n_dma_sems
                nwait = 0
                for op in self.ops[engname]:
                    need_e = {}
                    need_d = {}
                    for d in op.deps:
                        if d.is_dma:
                            if d.dma_val > known_d[d.dma_sem]:
                                if d.dma_val > need_d.get(d.dma_sem, 0):
                                    need_d[d.dma_sem] = d.dma_val
                        else:
                            if d.semval > known_e[d.eng]:
                                if d.semval > need_e.get(d.eng, 0):
                                    need_e[d.eng] = d.semval
                    if op.is_dma and op.dma_val - 16 > known_d[op.dma_sem]:
                        if op.dma_val - 16 > need_d.get(op.dma_sem, 0):
                            need_d[op.dma_sem] = op.dma_val - 16
                    for e, v in need_e.items():
                        engobj.wait_ge(esem[e], v)
                        known_e[e] = v
                        nwait += 1
                    for s, v in need_d.items():
                        engobj.wait_ge(dsem[s], v)
                        known_d[s] = v
                        nwait += 1
                    ins = getattr(engobj, op.fn[0])(*op.fn[1], **op.fn[2])
                    if self.annotate and op.phase:
                        ins.annotate(op.phase)
                    if op.is_dma:
                        ins.then_inc(dsem[op.dma_sem], 16)
                    elif op.mark:
                        ins.then_inc(esem[engname], 1)
                if engname == "sp":
                    for op in final_wait_ops:
                        if op.is_dma:
                            if op.dma_val > known_d[op.dma_sem]:
                                engobj.wait_ge(dsem[op.dma_sem], op.dma_val)
                                known_d[op.dma_sem] = op.dma_val
                        else:
                            if op.semval > known_e[op.eng]:
                                engobj.wait_ge(esem[op.eng], op.semval)
                                known_e[op.eng] = op.semval
                self.stats[engname] = (len(self.ops[engname]), nwait)

            @block.tensor
            def _(eng):
                run("pe", eng)

            @block.scalar
            def _(eng):
                run("act", eng)

            @block.vector
            def _(eng):
                run("dve", eng)

            @block.gpsimd
            def _(eng):
                run("pool", eng)

            @block.sync
            def _(eng):
                run("sp", eng)


def wlayout():
    off = 0
    L = {}

    def add(key, n):
        nonlocal off
        L[key] = (off, n)
        off += n
    for l in range(4):
        for f in range(2):
            for j in range(NJ):
                add(("gu", l, f, j), 2048)
            for c in range(8):
                add(("d", l, f, c), 2816)
        if l % 2 == 0:
            e = l // 2
            for c in range(4):
                add(("lru", e, c), 2048)
            for c in range(4):
                add(("qk", e, c), 2048)
            for vh in range(2):
                add(("v", e, vh), 2048)
        else:
            o = l // 2
            for c in range(8):
                add(("cin", o, c), 3072)
        for g in range(4):
            add(("out", l, g), 2048)
    return L, off


def playout():
    off = 0
    L = {}

    def add(key, n=1):
        nonlocal off
        L[key] = off
        off += n
    for l in range(4):
        for w in range(3):
            add(("pre", l, w), 8)
            add(("post", l, w), 8)
    for e in range(2):
        for j in range(4):
            add(("lcw", e, j), 4)
        add(("lcb", e), 4)
        add(("ba", e), 4)
        add(("bx", e), 4)
        add(("lam", e), 4)
    for o in range(2):
        for j in range(3):
            add(("ccw", o, j), 8)
    return L, off


def _chunkT(W, col):
    return W[:, col:col + 128].reshape(8, 128, 128).transpose(1, 0, 2)


def pack_weights(inp):
    L, tot = wlayout()
    wall = np.empty((128, tot), np.float32)

    def put(key, arr):
        o, n = L[key]
        wall[:, o:o + n] = arr.reshape(128, n)
    for l in range(4):
        for f in range(2):
            if f == 0:
                wg, wu, wd = inp["ffn1_w_gate"][l], inp["ffn1_w_up"][l], inp["ffn1_w_down"][l]
            else:
                wg, wu, wd = inp["ffn2_w_gate"][l], inp["ffn2_w_up"][l], inp["ffn2_w_down"][l]
            for j in range(NJ):
                put(("gu", l, f, j), np.stack([_chunkT(wg, j * 128), _chunkT(wu, j * 128)], axis=1))
            for c in range(8):
                put(("d", l, f, c), wd[:, c * 128:(c + 1) * 128].reshape(NJ, 128, 128).transpose(1, 0, 2))
        if l % 2 == 0:
            e = l // 2
            win = inp["ab_w_in"][e]
            wout = inp["ab_w_out"][e]
            for c in range(4):
                put(("lru", e, c), np.stack([_chunkT(win, 2048 + c * 128), _chunkT(win, 1536 + c * 128)], axis=1))
                put(("qk", e, c), np.stack([_chunkT(win, c * 128), _chunkT(win, 512 + c * 128)], axis=1))
            for vh in range(2):
                put(("v", e, vh), win[:, 1024 + vh * 256:1024 + (vh + 1) * 256].reshape(8, 128, 256).transpose(1, 0, 2))
        else:
            o = l // 2
            win = inp["c_w_in"][o]
            wout = inp["c_w_out"][o]
            for c in range(8):
                put(("cin", o, c), np.stack([_chunkT(win, 1024 + c * 128), _chunkT(win, 2048 + c * 128),
                                             _chunkT(win, c * 128)], axis=1))
        for g in range(4):
            put(("out", l, g), np.stack([_chunkT(wout, (2 * g) * 128), _chunkT(wout, (2 * g + 1) * 128)], axis=1))
    return wall


def pack_small(inp):
    L, n = playout()
    pv = np.zeros((128, n), np.float32)

    def colmajor(v):
        return v.reshape(-1, 128).T
    for l in range(4):
        for w in range(3):
            pv[:, L[("pre", l, w)]:L[("pre", l, w)] + 8] = colmajor(inp["norm_pre"][l, w])
            pv[:, L[("post", l, w)]:L[("post", l, w)] + 8] = colmajor(inp["norm_post"][l, w])
    for e in range(2):
        for j in range(4):
            pv[:, L[("lcw", e, j)]:L[("lcw", e, j)] + 4] = colmajor(inp["lru_conv_w"][e, j])
        pv[:, L[("lcb", e)]:L[("lcb", e)] + 4] = colmajor(inp["lru_conv_b"][e])
        pv[:, L[("ba", e)]:L[("ba", e)] + 4] = colmajor(inp["lru_gate_a_b"][e].reshape(-1))
        pv[:, L[("bx", e)]:L[("bx", e)] + 4] = colmajor(inp["lru_gate_x_b"][e].reshape(-1))
        pv[:, L[("lam", e)]:L[("lam", e)] + 4] = colmajor(inp["lru_lambda"][e])
    for o in range(2):
        for j in range(3):
            pv[:, L[("ccw", o, j)]:L[("ccw", o, j)] + 8] = colmajor(inp["c_conv_w"][o, j])
    wsm = np.zeros((128, 16, 128), np.float32)
    for e in range(2):
        for gi, nm in enumerate(("lru_gate_a_w", "lru_gate_x_w")):
            w = inp[nm][e]
            for c in range(4):
                idx = (e * 2 + gi) * 4 + c
                wsm[0:64, idx, 0:64] = w[2 * c]
                wsm[64:128, idx, 64:128] = w[2 * c + 1]
    return pv, wsm.reshape(128, 16 * 128)


def const_tables():
    cm = np.zeros((128, 4, 128), np.float32)
    cm[:, 0, :] = np.eye(128, dtype=np.float32)
    cm[:, 1, :] = 1.0
    for m in range(128):
        j = m % 64
        if j < 8:
            cm[m + 8, 2, m] = 1.0
        elif j < 16:
            cm[m - 8, 2, m] = 1.0
    k = np.arange(128)
    cm[:, 3, :] = (k[:, None] <= k[None, :]).astype(np.float32)
    inv_freq = (np.float32(ROPE_THETA) ** (-np.arange(0, 16, 2, dtype=np.float32) / np.float32(16))).astype(np.float32)
    pos = np.arange(SEQ, dtype=np.float32)
    ang = (pos[:, None] * inv_freq[None, :]).astype(np.float32).astype(np.float64)
    rope = np.zeros((128, 2, SEQ), np.float32)
    rope[:, 0, :] = 1.0
    for p in range(128):
        j = p % 64
        if j < 16:
            i = j % 8
            rope[p, 0, :] = np.cos(ang[:, i])
            rope[p, 1, :] = (-np.sin(ang[:, i])) if j < 8 else np.sin(ang[:, i])
    return cm.reshape(128, 4 * 128), rope.reshape(128, 2 * SEQ)


class Builder:
    def __init__(self, nseq=2, layers=(0, 1, 2, 3), nhalf=2, dbg=False, parts=("ffn1", "mixer", "ffn2")):
        self.dbg = dbg
        self.parts = parts
        self.stop_after = None
        self.wtot_override = None
        self.nseq = nseq
        self.layers = tuple(layers)
        self.nhalf = nhalf
        self.WL, self.WTOT = wlayout()
        self.PL, self.NP = playout()

    def sb(self, name, shape, dt):
        return self.st.enter_context(self.nc.sbuf_tensor(name, shape, dt))

    def ps(self, hold=False):
        while True:
            i = self.ps_set[self.ps_next % len(self.ps_set)]
            self.ps_next += 1
            if i not in self.ps_held:
                break
        if hold:
            self.ps_held.add(i)
        return self.PS[i], self.bPS[i]

    def ps_release(self, t):
        for i in range(8):
            if self.PS[i] is t:
                self.ps_held.discard(i)

    def rot(self, poolname):
        tiles, bufs, st = self.pools[poolname]
        i = st[0] % len(tiles)
        st[0] += 1
        return tiles[i], bufs[i]

    def mkpool(self, name, n, shape, dt):
        tiles = [self.sb("%s%d" % (name, i), shape, dt) for i in range(n)]
        bufs = [Buf("%s%d" % (name, i)) for i in range(n)]
        self.pools[name] = (tiles, bufs, [0])

    def wload(self, key):
        off, n = self.WL[key]
        i = self.w_next % N_WSLOT
        self.w_next += 1
        t, b = self.WS[i], self.bWS[i]
        self.P.dma("pool", M("dma_start", out=t[:, 0:n], in_=self.wall[:, off:off + n]),
                   writes=[b])
        return t, b

    def pcol(self, key, c=0):
        o = self.PL[key] + c
        return self.pvec[:, o:o + 1]

    def arena_reset(self):
        last = {}
        dmas = []
        for b in self.arena_bufs:
            ops = list(b.r)
            if b.w is not None:
                ops.append(b.w)
            for op in ops:
                if op.is_dma:
                    dmas.append(op)
                else:
                    cur = last.get(op.eng)
                    if cur is None or op.idx > cur.idx:
                        last[op.eng] = op
        self.arena_haz = list(last.values()) + dmas
        self.arena_bufs = []

    def abuf(self, name):
        b = Buf(name)
        b.r = list(self.arena_haz)
        self.arena_bufs.append(b)
        return b

    def build(self):
        nc = bass.Bass("TRN2", target_bir_lowering=False)
        self.nc = nc
        self.xs = nc.dram_tensor("xs", [self.nseq, SEQ, DM], F32, kind="ExternalInput").ap()
        self.wall = nc.dram_tensor("wall", [128, self.wtot_override or self.WTOT], F32, kind="ExternalInput").ap()
        self.pv_d = nc.dram_tensor("pv_in", [128, self.NP], F32, kind="ExternalInput").ap()
        self.wsm_d = nc.dram_tensor("wsm_in", [128, 2048], F32, kind="ExternalInput").ap()
        self.cm_d = nc.dram_tensor("cm_in", [128, 512], F32, kind="ExternalInput").ap()
        self.rope_d = nc.dram_tensor("rope_in", [128, 2 * SEQ], F32, kind="ExternalInput").ap()
        self.ys = nc.dram_tensor("ys", [self.nseq, SEQ, DM], F32, kind="ExternalOutput").ap()
        if self.dbg:
            self.dbg_d = nc.dram_tensor("dbg", [2, 128, 8 * SEG], F32, kind="ExternalOutput").ap()
        self.P = Prog(nc)
        self.pools = {}
        self.finals = []
        with ExitStack() as st:
            self.st = st
            self.alloc()
            self.setup()
            for s in range(self.nseq):
                self.seq_reset()
                for half in range(self.nhalf):
                    self.load_x(s, half)
                    sched = []
                    for l in self.layers:
                        if "ffn1" in self.parts:
                            sched.append(("ffn", l, 0))
                        if "mixer" in self.parts:
                            sched.append(("mix", l, 1))
                        if "ffn2" in self.parts:
                            sched.append(("ffn", l, 2))
                    if sched:
                        self.prenorm(sched[0][1], sched[0][2])
                    for i, (kind, l, w) in enumerate(sched):
                        self.next_norm = (sched[i + 1][1], sched[i + 1][2]) if i + 1 < len(sched) else None
                        if kind == "ffn":
                            self.ffn(l, w // 2, half)
                        elif l % 2 == 0:
                            self.mixer_even(l, half)
                        else:
                            self.mixer_odd(l, half)
                    self.store_x(s, half)
            self.P.emit(final_wait_ops=self.finals)
        return nc

    def alloc(self):
        nc = self.nc
        sb = self.sb
        self.X = sb("X", [128, 8, SEG], F32)
        self.bX = [[Buf("X%d_%d" % (c, t)) for t in range(2)] for c in range(8)]
        self.KTs = [sb("KTs%d" % e, [128, 4, SEG], BF16) for e in range(2)]
        self.bKTs = [[Buf("KTs%d_%d" % (e, c)) for c in range(4)] for e in range(2)]
        self.Vs = [sb("Vs%d" % e, [128, 8, 520], BF16) for e in range(2)]
        self.bVs = [[Buf("Vs%d_%d" % (e, k)) for k in range(8)] for e in range(2)]
        self.pvec = sb("pvec", [128, self.NP], F32)
        self.bpv = Buf("pvec")
        self.wsm = sb("wsm", [128, 16, 128], BF16)
        self.bwsm = Buf("wsm")
        self.cb = sb("cb", [128, 4, 128], BF16)
        self.bcb = Buf("cb")
        self.identf = sb("identf", [128, 128], F32)
        self.bident = Buf("identf")
        self.lc = sb("lc", [128, 2, 4, 2], F32)
        self.blc = Buf("lc")
        self.zh = sb("zh", [128, 2, 8, 2], F32)
        self.bzh = [[Buf("zh%d_%d" % (o, c)) for c in range(8)] for o in range(2)]
        self.lh = sb("lh", [128, 2, 4, 4], F32)
        self.blh = [[Buf("lh%d_%d" % (e, c)) for c in range(4)] for e in range(2)]
        self.hst = sb("hst", [128, 2, 4], F32)
        self.bhst = [[Buf("hst%d_%d" % (e, c)) for c in range(4)] for e in range(2)]
        self.kmx = sb("kmx", [128, 2, 4], F32)
        self.bkmx = [Buf("kmx%d" % e) for e in range(2)]
        self.small = sb("small", [128, 64], F32)
        self.H = sb("H", [128, 8, SEG], BF16)
        self.Ysb = self.H[:].rearrange("p c t -> p (c t)").bitcast(F32)
        self.bH = [[Buf("H%d_%d" % (c, t)) for t in range(2)] for c in range(8)]
        self.WS = [sb("ws%d" % i, [128, WSLOT], BF16) for i in range(N_WSLOT)]
        self.bWS = [Buf("ws%d" % i) for i in range(N_WSLOT)]
        self.w_next = 0
        self.mkpool("t32", 5, [128, TT], F32)
        self.mkpool("tb", 4, [128, TT], BF16)
        self.mkpool("rs", 2, [128, TT], F32)
        self.AR_BYTES = 77 * 1024
        self.AR = sb("arena", [128, self.AR_BYTES // 2], BF16)
        self.arena_bufs = []
        self.arena_haz = []
        self.PS = [self.st.enter_context(nc.psum_tensor("ps%d" % i, [128, TT], F32)) for i in range(8)]
        self.bPS = [Buf("ps%d" % i) for i in range(8)]
        self.ps_set = list(range(8))
        self.ps_next = 0
        self.ps_held = set()
        self.pending_stats = []

    def arena_view(self, byte_off, shape, dt):
        n = int(np.prod(shape[1:]))
        esz = 2 if dt == BF16 else 4
        assert byte_off % 4 == 0
        assert byte_off + n * esz <= self.AR_BYTES, (byte_off, n, esz)
        v = self.AR[:, byte_off // 2: byte_off // 2 + n * esz // 2]
        if dt == F32:
            v = v.bitcast(F32)
        return v

    def setup(self):
        P = self.P
        P.dma("sp", M("dma_start", out=self.pvec[:], in_=self.pv_d), writes=[self.bpv])
        P.dma("sp", M("dma_start", out=self.identf[:], in_=self.cm_d[:, 0:128]), writes=[self.bident])
        P.dma("pool", M("dma_start", out=self.wsm[:], in_=self.wsm_d.rearrange("p (a b) -> p a b", a=16)),
              writes=[self.bwsm])
        P.dma("pool", M("dma_start", out=self.cb[:], in_=self.cm_d.rearrange("p (a b) -> p a b", a=4)),
              writes=[self.bcb])
        bsm = Buf("small")
        for e in range(2):
            lam = self.pvec[:, self.PL[("lam", e)]:self.PL[("lam", e)] + 4]
            t1 = self.small[:, 0:4]
            t2 = self.small[:, 4:8]
            P.act(M("activation", out=t1, in_=lam, func=AF.Exp, scale=-1.0),
                  reads=[self.bpv], writes=[bsm])
            P.act(M("activation", out=t2, in_=t1, func=AF.Ln, bias=1.0),
                  reads=[bsm], writes=[bsm])
            P.dve(M("tensor_scalar", out=self.lc[:, e, :, 0], in0=t2, scalar1=-8.0, scalar2=None,
                                                         op0=ALU.mult), reads=[bsm], writes=[self.blc])
            P.dve(M("tensor_scalar", out=self.lc[:, e, :, 1], in0=t2, scalar1=-16.0, scalar2=None,
                                                         op0=ALU.mult), reads=[bsm], writes=[self.blc])
        for e in range(2):
            v4 = self.Vs[e][:].rearrange("p k (h d) -> p k h d", h=8)
            P.dve(M("memset", v4[:, :, :, 64:65], 1.0), writes=self.bVs[e])

    def seq_reset(self):
        P = self.P
        for o in range(2):
            P.dve(M("memset", self.zh[:, o], 0.0), writes=self.bzh[o])
        for e in range(2):
            P.dve(M("memset", self.lh[:, e], 0.0), writes=self.blh[e])
            P.dve(M("memset", self.hst[:, e], 0.0), writes=self.bhst[e])

    def stage_tiles(self):
        self.arena_reset()
        tiles = [self.arena_view(i * 4096, [128, DM], F32) for i in range(2)]
        bufs = [self.abuf("stage%d" % i) for i in range(2)]
        return tiles, bufs

    def load_x(self, s, half):
        P = self.P
        P.phase = "io"
        stiles, sbufs = self.stage_tiles()
        for tk in range(8):
            t0 = half * SEG + tk * 128
            stg, bst = stiles[tk % 2], sbufs[tk % 2]
            P.dma("sp", M("dma_start", out=stg, in_=self.xs[s, t0:t0 + 128, :]), writes=[bst])
            for cg in range(2):
                pt, bpt = self.ps()
                for ci in range(4):
                    c = cg * 4 + ci
                    P.pe(M("transpose", out=pt[:, ci * 128:(ci + 1) * 128],
                                                                         in_=stg[:, c * 128:(c + 1) * 128],
                                                                         identity=self.identf[:]),
                         reads=[bst, self.bident], writes=[bpt], accum=(ci > 0))
                tt = tk // 4
                col = tk * 128
                eng = P.act if cg == 0 else P.dve
                if cg == 0:
                    P.act(M("activation",
                        out=self.X[:, cg * 4:(cg + 1) * 4, col:col + 128],
                        in_=pt[:].rearrange("p (a b) -> p a b", a=4), func=AF.Copy),
                        reads=[bpt], writes=[self.bX[c][tt] for c in range(cg * 4, cg * 4 + 4)])
                else:
                    P.dve(M("tensor_copy",
                        out=self.X[:, cg * 4:(cg + 1) * 4, col:col + 128],
                        in_=pt[:].rearrange("p (a b) -> p a b", a=4)),
                        reads=[bpt], writes=[self.bX[c][tt] for c in range(cg * 4, cg * 4 + 4)])

    def store_x(self, s, half):
        P = self.P
        P.phase = "io"
        stiles, sbufs = self.stage_tiles()
        for tk in range(8):
            t0 = half * SEG + tk * 128
            tt = tk // 4
            col = tk * 128
            stg, bst = stiles[tk % 2], sbufs[tk % 2]
            for cg in range(2):
                pt, bpt = self.ps()
                for ci in range(4):
                    c = cg * 4 + ci
                    P.pe(M("transpose", out=pt[:, ci * 128:(ci + 1) * 128],
                                                                         in_=self.X[:, c, col:col + 128],
                                                                         identity=self.identf[:]),
                         reads=[self.bX[c][tt], self.bident], writes=[bpt], accum=(ci > 0))
                if cg == 0:
                    P.act(M("activation", out=stg[:, 0:512], in_=pt[:], func=AF.Copy),
                          reads=[bpt], writes=[bst])
                else:
                    P.dve(M("tensor_copy", out=stg[:, 512:1024], in_=pt[:]),
                          reads=[bpt], writes=[bst])
            d = P.dma("sp", M("dma_start", out=self.ys[s, t0:t0 + 128, :], in_=stg), reads=[bst])
            self.finals.append(d)

    def rstd_from(self, sps, bsps, hw):
        P = self.P
        sd, bsd = self.rot("t32")
        P.act(M("activation", out=sd[:], in_=sps[:], func=AF.Ln, scale=1.0 / DM, bias=EPS), reads=[bsps], writes=[bsd])
        rs, brs = self.rot("rs")
        P.act(M("activation", out=rs[:], in_=sd[:], func=AF.Exp, scale=-0.5, bias=float(np.log(hw))), reads=[bsd], writes=[brs])
        return rs, brs

    def prenorm_tt(self, l, w, tt):
        P = self.P
        ph = P.phase
        P.phase = "pre"
        tsl = slice(tt * TT, (tt + 1) * TT)
        sps, bsps = self.ps(hold=True)
        for c in range(8):
            sq, bsq = self.rot("tb")
            P.act(M("activation", out=sq[:], in_=self.X[:, c, tsl], func=AF.Square), reads=[self.bX[c][tt]], writes=[bsq])
            P.pe(M("matmul", sps[:], lhsT=self.cb[:, 1, :], rhs=sq[:], start=(c == 0), stop=(c == 7)),
                 reads=[bsq, self.bcb], writes=[bsps], accum=(c > 0))
        rs, brs = self.rstd_from(sps, bsps, 1.0)
        self.ps_release(sps)
        for c in range(8):
            g = self.pcol(("pre", l, w), c)
            t, bt = self.rot("t32")
            P.act(M("activation", out=t[:], in_=self.X[:, c, tsl], func=AF.Copy, scale=g), reads=[self.bX[c][tt], self.bpv], writes=[bt])
            P.dve(M("tensor_tensor", out=self.H[:, c, tsl], in0=t[:], in1=rs[:], op=ALU.mult), reads=[bt, brs], writes=[self.bH[c][tt]])
        P.phase = ph

    def prenorm(self, l, w):
        for tt in range(2):
            self.prenorm_tt(l, w, tt)

    def finish(self, l, w, spss, hw):
        for tt in range(2):
            self.postnorm(l, w, tt, spss[tt][0], spss[tt][1], hw)
            if self.next_norm is not None:
                self.prenorm_tt(self.next_norm[0], self.next_norm[1], tt)

    def ybuf(self, tt, c):
        if tt == 0:
            return self.Ysb[:, c * TT:(c + 1) * TT], [self.bH[c][0], self.bH[c][1]]
        return self.Ysb2[:, c * TT:(c + 1) * TT], [self.bY2[c]]

    def evac_y(self, py, bpy, c, tt, sps, bsps):
        P = self.P
        yv, yb = self.ybuf(tt, c)
        P.dve(M("tensor_copy", out=yv, in_=py[:]), reads=[bpy], writes=yb)
        sq, bsq = self.rot("tb")
        P.act(M("activation", out=sq[:], in_=yv, func=AF.Square), reads=yb, writes=[bsq])
        self.pending_stats.append((M("matmul", sps[:], lhsT=self.cb[:, 1, :], rhs=sq[:], start=(c == 0), stop=(c == 7)),
                                   [bsq, self.bcb], [bsps], c > 0))

    def flush_stats(self, keep=0):
        while len(self.pending_stats) > keep:
            fn, rd, wr, acc = self.pending_stats.pop(0)
            self.P.pe(fn, reads=rd, writes=wr, accum=acc)

    def postnorm(self, l, w, tt, sps, bsps, hw):
        P = self.P
        tsl = slice(tt * TT, (tt + 1) * TT)
        rs, brs = self.rstd_from(sps, bsps, hw)
        self.ps_release(sps)
        for c in range(8):
            g = self.pcol(("post", l, w), c)
            t, bt = self.rot("t32")
            yv, yb = self.ybuf(tt, c)
            P.act(M("activation", out=t[:], in_=yv, func=AF.Copy, scale=g), reads=yb + [self.bpv], writes=[bt])
            P.dve(M("tensor_tensor", out=t[:], in0=t[:], in1=rs[:], op=ALU.mult), reads=[bt, brs], writes=[bt])
            P.dve(M("tensor_tensor", out=self.X[:, c, tsl], in0=self.X[:, c, tsl], in1=t[:], op=ALU.add),
                  reads=[self.bX[c][tt], bt], writes=[self.bX[c][tt]])

    def make_ysb2(self, byte_off):
        self.Ysb2 = self.arena_view(byte_off, [128, 8 * TT], F32)
        haz = {}
        dmas = []
        for b in self.arena_bufs:
            ops = list(b.r)
            if b.w is not None:
                ops.append(b.w)
            for op in ops:
                if op.is_dma:
                    dmas.append(op)
                else:
                    cur = haz.get(op.eng)
                    if cur is None or op.idx > cur.idx:
                        haz[op.eng] = op
        hz = list(haz.values()) + dmas + list(self.arena_haz)
        self.bY2 = []
        for c in range(8):
            b = Buf("Y2_%d" % c)
            b.r = list(hz)
            self.arena_bufs.append(b)
            self.bY2.append(b)

    def ffn(self, l, f, half):
        P = self.P
        w = 0 if f == 0 else 2
        self.ps_set = list(range(8))
        P.phase = "ffnP1"
        self.arena_reset()
        A = self.arena_view(0, [128, NJ, SEG], BF16).rearrange("p (j t) -> p j t", j=NJ)
        bA = [[self.abuf("A%d_%d" % (j, t)) for t in range(2)] for j in range(NJ)]
        for jp in range(NJ // 2):
            wvs = []
            for j in (2 * jp, 2 * jp + 1):
                wt, bw = self.wload(("gu", l, f, j))
                wvs.append((j, wt[:, 0:2048].rearrange("p (s c f) -> p s c f", s=2, c=8), bw))
            for tt in range(2):
                tsl = slice(tt * TT, (tt + 1) * TT)
                for j, wv, bw in wvs:
                    pg, bg = self.ps()
                    pu, bu = self.ps()
                    for s_, (pp, bp) in enumerate(((pg, bg), (pu, bu))):
                        for c in range(8):
                            P.pe(M("matmul", pp[:], lhsT=wv[:, s_, c, :], rhs=self.H[:, c, tsl], start=(c == 0), stop=(c == 7)),
                                 reads=[bw, self.bH[c][tt]], writes=[bp], accum=(c > 0))
                    sg, bsg = self.rot("t32")
                    P.act(M("activation", out=sg[:], in_=pg[:], func=AF.Silu), reads=[bg], writes=[bsg])
                    P.dve(M("tensor_tensor", out=A[:, j, tsl], in0=sg[:], in1=pu[:], op=ALU.mult),
                          reads=[bsg, bu], writes=[bA[j][tt]])
        if self.stop_after == "phase1":
            return
        P.phase = "ffnP2"
        self.make_ysb2(NJ * SEG * 2)
        spss = [self.ps(hold=True) for _ in range(2)]
        order = [(c, tt) for c in range(4) for tt in range(2)] + [(c, 0) for c in range(4, 8)] + [(c, 1) for c in range(4, 8)]
        wmap = {}
        for c, tt in order:
            if c not in wmap:
                wt, bw = self.wload(("d", l, f, c))
                wmap[c] = (wt[:, 0:2816].rearrange("p (j f) -> p j f", j=NJ), bw)
            wv, bw = wmap[c]
            tsl = slice(tt * TT, (tt + 1) * TT)
            py, bpy = self.ps()
            for j in range(NJ):
                P.pe(M("matmul", py[:], lhsT=wv[:, j, :], rhs=A[:, j, tsl], start=(j == 0), stop=(j == NJ - 1)),
                     reads=[bw, bA[j][tt]], writes=[bpy], accum=(j > 0))
            self.flush_stats()
            self.evac_y(py, bpy, c, tt, spss[tt][0], spss[tt][1])
        self.flush_stats()
        self.finish(l, w, spss, 0.5)

    def out_proj(self, l, CAT, bCAT):
        P = self.P
        P.phase = "oproj"
        self.make_ysb2(16384)
        spss = [self.ps(hold=True) for _ in range(2)]
        order = [(cc, tt) for cc in range(4) for tt in range(2)] + [(cc, 0) for cc in range(4, 8)] + [(cc, 1) for cc in range(4, 8)]
        wmap = {}
        for cc, tt in order:
            g, s_ = cc // 2, cc % 2
            if g not in wmap:
                wt, bw = self.wload(("out", l, g))
                wmap[g] = (wt[:, 0:2048].rearrange("p (s c f) -> p s c f", s=2, c=8), bw)
            wv, bw = wmap[g]
            tsl = slice(tt * TT, (tt + 1) * TT)
            py, bpy = self.ps()
            for c in range(8):
                P.pe(M("matmul", py[:], lhsT=wv[:, s_, c, :], rhs=CAT[:, c, tsl], start=(c == 0), stop=(c == 7)),
                     reads=[bw, bCAT[c][tt]], writes=[bpy], accum=(c > 0))
            self.flush_stats()
            self.evac_y(py, bpy, cc, tt, spss[tt][0], spss[tt][1])
        self.flush_stats()
        self.finish(l, 1, spss, 1.0)

    def mixer_odd(self, l, half):
        P = self.P
        o = l // 2
        self.ps_set = list(range(8))
        P.phase = "odd"
        self.arena_reset()
        CAT = self.arena_view(0, [128, 8, SEG], BF16).rearrange("p (c t) -> p c t", c=8)
        bCAT = [[self.abuf("CAT%d_%d" % (c, t)) for t in range(2)] for c in range(8)]
        Z = [self.arena_view(16384 + i * 2064, [128, 516], F32) for i in range(2)]
        bZ = [self.abuf("Z%d" % i) for i in range(2)]
        zi = 0
        for c in range(8):
            wt, bw = self.wload(("cin", o, c))
            wv = wt[:, 0:3072].rearrange("p (s c f) -> p s c f", s=3, c=8)
            for tt in range(2):
                tsl = slice(tt * TT, (tt + 1) * TT)
                pps = [self.ps() for _ in range(3)]
                for s_ in range(3):
                    pp, bp = pps[s_]
                    for k in range(8):
                        P.pe(M("matmul",
                            pp[:], lhsT=wv[:, s_, k, :], rhs=self.H[:, k, tsl], start=(k == 0), stop=(k == 7)),
                            reads=[bw, self.bH[k][tt]], writes=[bp], accum=(k > 0))
                (pc, bpc), (px, bpx), (pb, bpb) = pps
                z, bz = Z[zi % 2], bZ[zi % 2]
                zi += 1
                zc, bzc = self.rot("t32")
                P.act(M("activation", out=zc[:], in_=pc[:], func=AF.Copy), reads=[bpc], writes=[bzc])
                P.dve(M("tensor_copy", out=z[:, 0:2], in_=self.zh[:, o, c, :]), reads=[self.bzh[o][c]], writes=[bz])
                P.dve(M("tensor_tensor", out=z[:, 2:514], in0=zc[:], in1=px[:], op=ALU.mult),
                      reads=[bzc, bpx, bz], writes=[bz])
                P.dve(M("tensor_copy", out=self.zh[:, o, c, :], in_=z[:, 512:514]), reads=[bz], writes=[self.bzh[o][c]])
                y, by = self.rot("t32")
                P.dve(M("tensor_scalar", out=y[:], in0=z[:, 0:512], scalar1=self.pcol(("ccw", o, 0), c),
                                                           scalar2=None, op0=ALU.mult), reads=[bz, self.bpv], writes=[by])
                for j in (1, 2):
                    P.dve(M("scalar_tensor_tensor",
                        out=y[:], in0=z[:, j:j + 512], scalar=self.pcol(("ccw", o, j), c), in1=y[:], op0=ALU.mult, op1=ALU.add),
                        reads=[bz, by, self.bpv], writes=[by])
                P.dve(M("tensor_tensor", out=CAT[:, c, tsl], in0=y[:], in1=pb[:], op=ALU.mult),
                      reads=[by, bpb], writes=[bCAT[c][tt]])
        self.out_proj(l, CAT, bCAT)

    def mixer_even(self, l, half):
        P = self.P
        e_ = l // 2
        self.ps_set = list(range(8))
        P.phase = "lru"
        self.arena_reset()
        off = [0]

        def carve(shape, dt):
            n = int(np.prod(shape[1:])) * (2 if dt == BF16 else 4)
            v = self.arena_view(off[0], shape, dt)
            off[0] += (n + 3) // 4 * 4
            return v
        CAT = carve([128, 8, SEG], BF16).rearrange("p (c t) -> p c t", c=8)
        bCAT = [[self.abuf("CAT%d_%d" % (c, t)) for t in range(2)] for c in range(8)]
        QT = carve([128, 4, SEG], BF16).rearrange("p (c t) -> p c t", c=4)
        bQT = [[self.abuf("QT%d_%d" % (c, t)) for t in range(2)] for c in range(4)]
        ropet = carve([128, 2, SEG], F32).rearrange("p (a t) -> p a t", a=2)
        brope = self.abuf("rope")
        if half == 0:
            KTc, bKTc = self.KTs[e_], self.bKTs[e_]
            Vc, bVc = self.Vs[e_], self.bVs[e_]
        else:
            KTc = carve([128, 4, SEG], BF16).rearrange("p (c t) -> p c t", c=4)
            bKTc = [self.abuf("KTc%d" % c) for c in range(4)]
            Vc = carve([128, 8, 520], BF16).rearrange("p (k d) -> p k d", k=8)
            bVc = [self.abuf("Vc%d" % k) for k in range(8)]
            v4 = Vc.rearrange("p k (h d) -> p k h d", h=8)
            P.dve(M("memset", v4[:, :, :, 64:65], 1.0), writes=bVc)
        XR = carve([128, 516], F32)
        bXR = self.abuf("XR")
        lt = [carve([128, TT], F32) for _ in range(6)]
        blt = [self.abuf("lt%d" % i) for i in range(6)]
        xcb = carve([128, TT], BF16)
        bxcb = self.abuf("xcb")
        NPT = 6
        PT = [carve([128, 256], BF16) for _ in range(NPT)]
        bPT = [self.abuf("PT%d" % i) for i in range(NPT)]
        osb = [carve([128, 8, 65], F32).rearrange("p (n d) -> p n d", n=8) for _ in range(2)]
        bosb = [self.abuf("osb%d" % i) for i in range(2)]
        atok = [carve([128, 512], F32) for _ in range(2)]
        batok = [self.abuf("atok%d" % i) for i in range(2)]
        acc = [carve([128, 68], F32) for _ in range(2)]
        bacc = [self.abuf("acc%d" % i) for i in range(2)]
        KMb = carve([128, 4, 8], BF16).rearrange("p (c n) -> p c n", c=4)
        bKM = self.abuf("KM")
        KMf = carve([128, 4, 8], F32).rearrange("p (c n) -> p c n", c=4)
        bKMf = self.abuf("KMf")
        g8 = [carve([128, 8], F32) for _ in range(4)]
        bg8 = [self.abuf("g8_%d" % i) for i in range(4)]
        top8 = [carve([128, 8], F32) for _ in range(4)]
        btop8 = [self.abuf("top8_%d" % i) for i in range(4)]
        sel = [carve([128, 8], F32) for _ in range(4)]
        bsel = [self.abuf("sel%d" % i) for i in range(4)]
        qm = carve([128, 4, 2], F32).rearrange("p (c t) -> p c t", c=4)
        bqm = self.abuf("qm")
        km = carve([128, 4, 2], F32).rearrange("p (c t) -> p c t", c=4)
        bkm = self.abuf("km")
        negM = carve([128, 4], F32)
        bnegM = self.abuf("negM")
        sm1 = carve([128, 4], F32)
        sm2 = carve([128, 4], F32)
        bsm = self.abuf("sm")
        rcp = [carve([128, 2], F32) for _ in range(2)]
        brcp = [self.abuf("rcp%d" % i) for i in range(2)]

        pos0 = half * SEG
        P.dma("sp", M("dma_start", out=ropet, in_=self.rope_d.rearrange("p (a t) -> p a t", a=2)[:, :, pos0:pos0 + SEG]),
              writes=[brope])

        gg, xc, r_, i_, a_, s2 = lt
        bgg, bxc, br, bi, ba, bs2 = blt
        for c in range(4):
            wt, bw = self.wload(("lru", e_, c))
            wv = wt[:, 0:2048].rearrange("p (s c f) -> p s c f", s=2, c=8)
            wtq, bwq = self.wload(("qk", e_, c))
            wvq = wtq[:, 0:2048].rearrange("p (s c f) -> p s c f", s=2, c=8)
            for tt in range(2):
                tsl = slice(tt * TT, (tt + 1) * TT)
                P.phase = "lru"
                pgg, bpg = self.ps()
                pxx, bpx = self.ps()
                for s_, (pp, bp) in enumerate(((pgg, bpg), (pxx, bpx))):
                    for k in range(8):
                        P.pe(M("matmul", pp[:], lhsT=wv[:, s_, k, :], rhs=self.H[:, k, tsl], start=(k == 0), stop=(k == 7)),
                             reads=[bw, self.bH[k][tt]], writes=[bp], accum=(k > 0))
                P.act(M("activation", out=gg[:], in_=pgg[:], func=AF.Gelu_apprx_tanh), reads=[bpg], writes=[bgg])
                P.dve(M("tensor_copy", out=XR[:, 0:3], in_=self.lh[:, e_, c, 0:3]), reads=[self.blh[e_][c]], writes=[bXR])
                P.act(M("activation", out=XR[:, 3:515], in_=pxx[:], func=AF.Copy), reads=[bpx, bXR], writes=[bXR])
                P.dve(M("tensor_copy", out=self.lh[:, e_, c, 0:3], in_=XR[:, 512:515]), reads=[bXR], writes=[self.blh[e_][c]])
                P.phase = "qk"
                pqk = []
                for s_ in range(2):
                    pq, bpq = self.ps()
                    for k in range(8):
                        P.pe(M("matmul", pq[:], lhsT=wvq[:, s_, k, :], rhs=self.H[:, k, tsl], start=(k == 0), stop=(k == 7)),
                             reads=[bwq, self.bH[k][tt]], writes=[bpq], accum=(k > 0))
                    qraw, bqraw = self.rot("tb")
                    P.act(M("activation", out=qraw[:], in_=pq[:], func=AF.Copy), reads=[bpq], writes=[bqraw])
                    pqk.append((pq, bpq, qraw, bqraw))
                P.phase = "lru"
                P.dve(M("tensor_scalar", out=xc[:], in0=XR[:, 0:512], scalar1=self.pcol(("lcw", e_, 0), c),
                        scalar2=self.pcol(("lcb", e_), c), op0=ALU.mult, op1=ALU.add), reads=[bXR, self.bpv], writes=[bxc])
                for j in (1, 2, 3):
                    P.dve(M("scalar_tensor_tensor", out=xc[:], in0=XR[:, j:j + 512], scalar=self.pcol(("lcw", e_, j), c), in1=xc[:],
                            op0=ALU.mult, op1=ALU.add), reads=[bXR, bxc, self.bpv], writes=[bxc])
                P.phase = "qk"
                prots = []
                for s_ in range(2):
                    pq, bpq, qraw, bqraw = pqk[s_]
                    prot, bprot = self.ps()
                    P.pe(M("matmul", prot[:], lhsT=self.cb[:, 2, :], rhs=qraw[:], start=True, stop=True),
                         reads=[bqraw, self.bcb], writes=[bprot])
                    prots.append((prot, bprot))
                P.phase = "lru"
                P.act(M("activation", out=xcb[:], in_=xc[:], func=AF.Copy), reads=[bxc], writes=[bxcb])
                pr, bpr = self.ps()
                pi, bpi = self.ps()
                P.pe(M("matmul", pr[:], lhsT=self.wsm[:, (e_ * 2 + 0) * 4 + c, :], rhs=xcb[:], start=True, stop=True),
                     reads=[bxcb, self.bwsm], writes=[bpr])
                P.pe(M("matmul", pi[:], lhsT=self.wsm[:, (e_ * 2 + 1) * 4 + c, :], rhs=xcb[:], start=True, stop=True),
                     reads=[bxcb, self.bwsm], writes=[bpi])
                P.phase = "qk"
                for s_ in range(2):
                    pq, bpq, qraw, bqraw = pqk[s_]
                    prot, bprot = prots[s_]
                    t1, bt1 = self.rot("t32")
                    t2, bt2 = self.rot("t32")
                    P.dve(M("tensor_tensor", out=t1[:], in0=pq[:], in1=ropet[:, 0, tsl], op=ALU.mult),
                          reads=[bpq, brope, bqraw], writes=[bt1])
                    P.dve(M("tensor_tensor", out=t2[:], in0=prot[:], in1=ropet[:, 1, tsl], op=ALU.mult),
                          reads=[bprot, brope], writes=[bt2])
                    if s_ == 0:
                        dst, bd = QT[:, c, tsl], bQT[c][tt]
                    else:
                        dst, bd = KTc[:, c, tsl], bKTc[c]
                    P.dve(M("tensor_tensor", out=dst, in0=t1[:], in1=t2[:], op=ALU.add), reads=[bt1, bt2], writes=[bd])
                P.phase = "lru"
                P.act(M("activation", out=r_[:], in_=pr[:], func=AF.Sigmoid, bias=self.pcol(("ba", e_), c)),
                      reads=[bpr, self.bpv], writes=[br])
                P.act(M("activation", out=i_[:], in_=pi[:], func=AF.Sigmoid, bias=self.pcol(("bx", e_), c)),
                      reads=[bpi, self.bpv], writes=[bi])
                P.act(M("activation", out=a_[:], in_=r_[:], func=AF.Exp, scale=self.lc[:, e_, c, 0:1]), reads=[br, self.blc], writes=[ba])
                P.act(M("activation", out=s2[:], in_=r_[:], func=AF.Exp, scale=self.lc[:, e_, c, 1:2]), reads=[br, self.blc], writes=[bs2])
                P.act(M("activation", out=s2[:], in_=s2[:], func=AF.Sqrt, scale=-1.0, bias=1.0), reads=[bs2], writes=[bs2])
                P.dve(M("tensor_tensor", out=i_[:], in0=i_[:], in1=xc[:], op=ALU.mult), reads=[bi, bxc], writes=[bi])
                P.dve(M("tensor_tensor", out=i_[:], in0=i_[:], in1=s2[:], op=ALU.mult), reads=[bi, bs2], writes=[bi])
                P.dve(M("tensor_tensor_scan", out=r_[:], data0=a_[:], data1=i_[:], initial=self.hst[:, e_, c:c + 1],
                        op0=ALU.mult, op1=ALU.add), reads=[ba, bi, self.bhst[e_][c], br], writes=[br])
                P.dve(M("tensor_copy", out=self.hst[:, e_, c:c + 1], in_=r_[:, TT - 1:TT]), reads=[br], writes=[self.bhst[e_][c]])
                P.dve(M("tensor_tensor", out=CAT[:, 4 + c, tsl], in0=r_[:], in1=gg[:], op=ALU.mult),
                      reads=[br, bgg], writes=[bCAT[4 + c][tt]])

        P.phase = "v"
        for vh in range(2):
            wt, bw = self.wload(("v", e_, vh))
            wv = wt[:, 0:2048].rearrange("p (c f) -> p c f", c=8)
            for tk in range(8):
                pv_, bpv_ = self.ps()
                for k in range(8):
                    P.pe(M("matmul",
                        pv_[:, 0:256], lhsT=self.H[:, k, tk * 128:(tk + 1) * 128], rhs=wv[:, k, :], start=(k == 0), stop=(k == 7)),
                        reads=[bw, self.bH[k][tk // 4]], writes=[bpv_], accum=(k > 0))
                dst = Vc[:, tk, :].rearrange("p (h d) -> p h d", h=8)[:, vh * 4:(vh + 1) * 4, 0:64]
                P.act(M("activation", out=dst, in_=pv_[:, 0:256].rearrange("p (h d) -> p h d", h=4), func=AF.Copy),
                      reads=[bpv_], writes=[bVc[tk]])

        P.phase = "attn"
        scale = 0.125
        for c in range(4):
            for which, (src, bsrc, dstm, bdm) in enumerate(((QT, None, qm, bqm), (KTc, None, km, bkm))):
                for tt in range(2):
                    tsl = slice(tt * TT, (tt + 1) * TT)
                    rb = [bQT[c][tt]] if which == 0 else [bKTc[c]]
                    sq, bsq = self.rot("tb")
                    P.act(M("activation", out=sq[:], in_=src[:, c, tsl], func=AF.Square),
                          reads=rb, writes=[bsq])
                    pn, bpn = self.ps()
                    P.pe(M("matmul", pn[:], lhsT=self.cb[:, 1, :], rhs=sq[:], start=True, stop=True),
                         reads=[bsq, self.bcb], writes=[bpn])
                    P.dve(M("tensor_reduce", out=dstm[:, c, tt:tt + 1], in_=pn[:], axis=AX.X, op=ALU.max),
                          reads=[bpn], writes=[bdm])
        P.dve(M("tensor_tensor", out=sm1[:], in0=qm[:, :, 0], in1=qm[:, :, 1], op=ALU.max), reads=[bqm], writes=[bsm])
        P.dve(M("tensor_tensor", out=sm2[:], in0=km[:, :, 0], in1=km[:, :, 1], op=ALU.max), reads=[bkm, bsm], writes=[bsm])
        if half == 0:
            P.dve(M("tensor_copy", out=self.kmx[:, e_, :], in_=sm2[:]), reads=[bsm], writes=[self.bkmx[e_]])
        else:
            P.dve(M("tensor_tensor", out=sm2[:], in0=sm2[:], in1=self.kmx[:, e_, :], op=ALU.max),
                  reads=[bsm, self.bkmx[e_]], writes=[bsm])
        P.dve(M("tensor_tensor", out=sm1[:], in0=sm1[:], in1=sm2[:], op=ALU.mult), reads=[bsm], writes=[bsm])
        P.act(M("activation", out=sm1[:], in_=sm1[:], func=AF.Sqrt), reads=[bsm], writes=[bsm])
        P.dve(M("tensor_scalar", out=negM[:], in0=sm1[:], scalar1=-scale, scalar2=None, op0=ALU.mult), reads=[bsm], writes=[bnegM])

        def kt_src(kc):
            if kc < 8 and half == 1:
                return self.KTs[e_], self.bKTs[e_], kc
            return KTc, bKTc, kc % 8

        def v_src(kc):
            if kc < 8 and half == 1:
                return self.Vs[e_], self.bVs[e_], kc
            return Vc, bVc, kc % 8

        if half == 1:
            for c in range(4):
                P.dve(M("tensor_reduce", out=KMf[:, c, 0:4], in_=self.KTs[e_][:, c, :].rearrange("p (n k) -> p n k", n=4),
                                                     axis=AX.X, op=ALU.add), reads=[self.bKTs[e_][c]], writes=[bKMf])
                P.dve(M("tensor_reduce", out=KMf[:, c, 4:8], in_=KTc[:, c, :].rearrange("p (n k) -> p n k", n=4),
                                                     axis=AX.X, op=ALU.add), reads=[bKTc[c]], writes=[bKMf])
            P.dve(M("tensor_scalar", out=KMb[:], in0=KMf[:], scalar1=1.0 / 256, scalar2=None, op0=ALU.mult),
                  reads=[bKMf], writes=[bKM])

        self.ps_set = [4, 5, 6, 7]
        OB = [self.PS[i] for i in range(4)]
        bOB = [self.bPS[i] for i in range(4)]
        pti = [0]
        LAG = 2
        for qg in range(4):
            own = 4 * half + qg
            q0 = qg * 256
            nkc = 2 * own + 2
            use_sel = own >= 4

            def emit_sel(hd):
                c = hd // 2
                hp = 64 * (hd % 2)
                par = hd % 2
                for qt in range(2):
                    k_ = qt * 2 + par
                    pgt, bpgt = self.ps()
                    P.pe(M("matmul", pgt[:, 0:8], lhsT=QT[hp:hp + 64, c, q0 + qt * 128:q0 + (qt + 1) * 128],
                           rhs=KMb[hp:hp + 64, c, :], start=True, stop=True), reads=[bQT[c][qg // 2], bKM], writes=[bpgt])
                    P.dve(M("memset", g8[k_][:], -1e30), writes=[bg8[k_]])
                    P.dve(M("tensor_copy", out=g8[k_][:, 0:own], in_=pgt[:, 0:own]), reads=[bpgt, bg8[k_]], writes=[bg8[k_]])
                    P.dve(M("max", out=top8[k_][:], in_=g8[k_][:]), reads=[bg8[k_]], writes=[btop8[k_]])
                    P.dve(M("tensor_scalar", out=sel[k_][:], in0=g8[k_][:], scalar1=top8[k_][:, 2:3], scalar2=None, op0=ALU.is_ge),
                          reads=[bg8[k_], btop8[k_]], writes=[bsel[k_]])
                    P.dve(M("memset", sel[k_][:, own:own + 1], 1.0), reads=[bsel[k_]], writes=[bsel[k_]])

            def emit_S(hd, kc):
                c = hd // 2
                hp = 64 * (hd % 2)
                ktt, bkt, kl = kt_src(kc)
                dq = kc - 2 * own
                qlo = 128 if dq == 1 else 0
                pS, bpS = self.ps(hold=True)
                P.pe(M("matmul", pS[:, qlo:256], lhsT=ktt[hp:hp + 64, c, kl * 128:(kl + 1) * 128],
                       rhs=QT[hp:hp + 64, c, q0 + qlo:q0 + 256], start=True, stop=True),
                     reads=[bkt[c], bQT[c][qg // 2]], writes=[bpS])
                return pS, bpS, qlo, dq

            def emit_EV(hd, kc, st_):
                pS, bpS, qlo, dq = st_
                c = hd // 2
                vt, bv, vl = v_src(kc)
                n = kc // 2
                pt_, bpt_ = PT[pti[0] % NPT], bPT[pti[0] % NPT]
                pti[0] += 1
                P.act(M("activation", out=pt_[:, qlo:256], in_=pS[:, qlo:256], func=AF.Exp, scale=scale, bias=negM[:, c:c + 1]),
                      reads=[bpS, bnegM], writes=[bpt_])
                self.ps_release(pS)
                if dq >= 0:
                    P.dve(M("tensor_tensor", out=pt_[:, qlo:qlo + 128], in0=pt_[:, qlo:qlo + 128], in1=self.cb[:, 3, :], op=ALU.mult),
                          reads=[bpt_, self.bcb], writes=[bpt_])
                for qt in range(2):
                    if qt * 128 < qlo:
                        continue
                    gq = 2 * own + qt
                    if use_sel:
                        reg = n
                        first = (kc % 2 == 0)
                        last = (kc % 2 == 1) or (kc == gq)
                        bank = qt * 2 + reg // 4
                    else:
                        reg = 0
                        first = (kc == 0)
                        last = (kc == gq)
                        bank = qt * 2 + hd % 2
                    ob, bob = OB[bank], bOB[bank]
                    P.pe(M("matmul", ob[:, (reg % 4) * 65:(reg % 4) * 65 + 65], lhsT=pt_[:, qt * 128:(qt + 1) * 128],
                           rhs=vt[:, vl, hd * 65:(hd + 1) * 65], start=first, stop=last),
                         reads=[bpt_, bv[vl]], writes=[bob], accum=(not first))

            def emit_combine(hd):
                par = hd % 2
                for qt in range(2):
                    ac, bac = acc[qt], bacc[qt]
                    if use_sel:
                        k_ = qt * 2 + par
                        nb = own + 1
                        P.dve(M("tensor_copy", out=osb[qt][:, 0:4, :], in_=OB[qt * 2][:, 0:260].rearrange("p (n d) -> p n d", n=4)),
                              reads=[bOB[qt * 2]], writes=[bosb[qt]])
                        P.dve(M("tensor_copy", out=osb[qt][:, 4:nb, :],
                                in_=OB[qt * 2 + 1][:, 0:(nb - 4) * 65].rearrange("p (n d) -> p n d", n=nb - 4)),
                              reads=[bOB[qt * 2 + 1], bosb[qt]], writes=[bosb[qt]])
                for qt in range(2):
                    ac, bac = acc[qt], bacc[qt]
                    if use_sel:
                        k_ = qt * 2 + par
                        nb = own + 1
                        P.dve(M("tensor_tensor", out=osb[qt][:, 0:nb, :], in0=osb[qt][:, 0:nb, :],
                                in1=sel[k_][:, 0:nb].unsqueeze(2).broadcast_to([128, nb, 65]), op=ALU.mult),
                              reads=[bosb[qt], bsel[k_]], writes=[bosb[qt]])
                        P.dve(M("tensor_reduce", out=ac[:, 0:65], in_=osb[qt][:, 0:nb, :].rearrange("p n d -> p d n"), axis=AX.X, op=ALU.add),
                              reads=[bosb[qt]], writes=[bac])
                    else:
                        ob, bob = OB[qt * 2 + par], bOB[qt * 2 + par]
                        P.dve(M("tensor_copy", out=ac[:, 0:65], in_=ob[:, 0:65]), reads=[bob], writes=[bac])
                    P.dve(M("reciprocal", out=rcp[qt][:, 0:1], in_=ac[:, 64:65]), reads=[bac], writes=[brcp[qt]])
                    P.dve(M("tensor_scalar", out=atok[qt][:, hd * 64:(hd + 1) * 64], in0=ac[:, 0:64], scalar1=rcp[qt][:, 0:1],
                            scalar2=None, op0=ALU.mult), reads=[bac, brcp[qt]], writes=[batok[qt]])

            seq_ = [(hd, kc) for hd in range(8) for kc in range(nkc)]
            states = {}
            for i in range(len(seq_) + LAG):
                if i < len(seq_):
                    hd, kc = seq_[i]
                    if kc == 0 and use_sel:
                        emit_sel(hd)
                    states[i] = emit_S(hd, kc)
                j = i - LAG
                if j >= 0:
                    hd, kc = seq_[j]
                    emit_EV(hd, kc, states.pop(j))
                    if kc == nkc - 1:
                        emit_combine(hd)
            for qt in range(2):
                ptp, bptp = self.ps()
                for cc in range(4):
                    P.pe(M("transpose", out=ptp[:, cc * 128:(cc + 1) * 128],
                                                                      in_=atok[qt][:, cc * 128:(cc + 1) * 128], identity=self.identf[:]),
                         reads=[batok[qt], self.bident], writes=[bptp], accum=(cc > 0))
                col = q0 + qt * 128
                P.act(M("activation", out=CAT[:, 0:4, col:col + 128],
                                                               in_=ptp[:].rearrange("p (a b) -> p a b", a=4), func=AF.Copy),
                      reads=[bptp], writes=[bCAT[cc][qg // 2] for cc in range(4)])
        self.ps_set = list(range(8))
        if self.dbg:
            d = P.dma("pool", M("dma_start", out=self.dbg_d[half].rearrange("p (c t) -> p c t", c=8), in_=CAT),
                      reads=[b for bb in bCAT for b in bb])
            self.finals.append(d)
        self.out_proj(l, CAT, bCAT)


_CACHE = {}


def _get_prog(nseq, layers, nhalf):
    key = (nseq, tuple(layers), nhalf)
    if key not in _CACHE:
        _CACHE[key] = Builder(nseq, layers, nhalf).build()
    return _CACHE[key]


def kernel(**inputs):
    inp = {k: np.asarray(v) for k, v in inputs.items()}
    x = np.ascontiguousarray(inp["x"], dtype=np.float32)
    wall = pack_weights(inp)
    pv, wsm = pack_small(inp)
    cm, rope = const_tables()
    nc = _get_prog(2, (0, 1, 2, 3), 2)
    in_maps = []
    for i in range(NCORES):
        in_maps.append({"xs": x[2 * i:2 * i + 2], "wall": wall, "pv_in": pv, "wsm_in": wsm, "cm_in": cm, "rope_in": rope})
    res = run_bass_kernel_spmd(nc, in_maps, core_ids=list(range(NCORES)))
    out = np.concatenate([r["ys"] for r in res.results], axis=0)
    return out.astype(np.float32)
```
